# Optimizing a Trainium2 kernel written in Bass

```python
import math
import jax
import jax.numpy as jnp
from jax import lax
import numpy as np

D_MODEL = 2048
BATCH = 4
SEQ = 2048
DEPTH = 1
DEC_BATCH = 32
DEC_SEQ = 8
PAST_LEN = 8192
PAGE_SIZE = 128

ATT_WIDTH = D_MODEL // 2
ATT_HEAD_DIM = 128
ATT_HEADS = ATT_WIDTH // ATT_HEAD_DIM
MOBA_BLOCK = 256
MOBA_TOPK = 3
QROWS = 128
ML_WIDTH = D_MODEL - ATT_WIDTH
ML_HEADS = 4
ML_HEAD_DIM = ML_WIDTH // ML_HEADS
ML_CHUNK = 64
IN_WIDTH = 3 * ATT_WIDTH + 4 * ML_WIDTH + 2 * ML_HEADS
N_GROUPS = 4
EXPERTS_PER_GROUP = 4
N_EXPERTS = N_GROUPS * EXPERTS_PER_GROUP
EXPERT_TOPK = 2
EXPERT_FF = D_MODEL // 4
N_MOD = 6
EPS = 1e-6

kernel_name = 'hymba_moba_mlstm_hmoe_step'


def rms_norm(x, g):
    xf = x.astype(jnp.float32)
    y = xf * lax.rsqrt(jnp.mean(xf * xf, axis=-1, keepdims=True) + EPS)
    return (y * g.astype(jnp.float32)).astype(x.dtype)


def alibi_slopes():
    return 2.0 ** (-8.0 * jnp.arange(1, ATT_HEADS + 1, dtype=jnp.float32) / ATT_HEADS)


def to_blocks(parts):
    B, _, H, hd = parts[0].shape
    L = sum(p.shape[1] for p in parts)
    pad = (-L) % MOBA_BLOCK
    if pad:
        parts = parts + [jnp.zeros((B, pad, H, hd), parts[0].dtype)]
    kb = jnp.concatenate(parts, axis=1) if len(parts) > 1 else parts[0]
    return kb.reshape(B, -1, MOBA_BLOCK, H, hd)


def moba_attention(q, k_blocks, v_blocks, q_pos, slopes):
    B, T, H, hd = q.shape
    NB = k_blocks.shape[1]
    topk = min(MOBA_TOPK, NB)
    k1 = topk + 1
    f32 = jnp.float32
    k_mean = jnp.mean(k_blocks, axis=2, dtype=f32)
    qc = math.gcd(T, max(1, QROWS // B))
    n_chunks = T // qc
    scale = hd ** -0.5
    bidx = jnp.arange(B)[:, None, None, None]
    hidx = jnp.arange(H)[None, :, None, None]
    blk_ids = jnp.arange(NB)
    rank = jnp.arange(topk)
    offs = jnp.arange(MOBA_BLOCK)

    def one_chunk(args):
        qq, pp = args
        cur = pp // MOBA_BLOCK
        gate = jnp.einsum('bqhd,bnhd->bhqn', qq.astype(f32), k_mean)
        gate = jnp.where(blk_ids[None, None, None, :] < cur[None, None, :, None], gate, -jnp.inf)
        _, sel = lax.top_k(gate, topk)
        sel_ok = jnp.broadcast_to(rank[None, None, None, :] < cur[None, None, :, None], (B, H, qc, topk))
        own = jnp.broadcast_to(cur[None, None, :, None], (B, H, qc, 1))
        blk = jnp.concatenate([sel.astype(jnp.int32), own.astype(jnp.int32)], axis=-1)
        ok = jnp.concatenate([sel_ok, jnp.ones((B, H, qc, 1), bool)], axis=-1)
        kg = k_blocks[bidx, blk, :, hidx, :]
        vg = v_blocks[bidx, blk, :, hidx, :]
        s = jnp.einsum('bqhd,bhqnkd->bhqnk', qq, kg, preferred_element_type=f32) * scale
        dist = pp[None, None, :, None, None] - (blk[..., None] * MOBA_BLOCK + offs)
        s = s - slopes[None, :, None, None, None] * dist.astype(f32)
        s = jnp.where(ok[..., None] & (dist >= 0), s, -jnp.inf)
        p = jax.nn.softmax(s.reshape(B, H, qc, k1 * MOBA_BLOCK), axis=-1)
        return jnp.einsum('bhqm,bhqmd->bqhd', p.astype(vg.dtype),
                          vg.reshape(B, H, qc, k1 * MOBA_BLOCK, hd))

    qs = q.reshape(B, n_chunks, qc, H, hd).transpose(1, 0, 2, 3, 4)
    ps = q_pos.reshape(n_chunks, qc)
    out = lax.map(one_chunk, (qs, ps))
    return out.transpose(1, 0, 2, 3, 4).reshape(B, T, H, hd)


def mlstm_chunk(carry, inp):
    C, n, m = carry
    q, k, v, ig, lf = inp
    L = q.shape[2]
    b = jnp.cumsum(lf, axis=-1)
    dmat = b[..., :, None] - b[..., None, :] + ig[..., None, :]
    causal = jnp.tril(jnp.ones((L, L), bool))
    dmat = jnp.where(causal, dmat, -jnp.inf)
    inter = b + m[..., None]
    mt = jnp.maximum(inter, jnp.max(dmat, axis=-1))
    w = jnp.exp(dmat - mt[..., None])
    a = jnp.exp(inter - mt)
    sc = jnp.einsum('bhtd,bhsd->bhts', q, k) * w
    num = a[..., None] * jnp.einsum('bhvk,bhtk->bhtv', C, q) + jnp.einsum('bhts,bhsv->bhtv', sc, v)
    den = a * jnp.einsum('bhk,bhtk->bht', n, q) + jnp.sum(sc, axis=-1)
    h = num / jnp.maximum(jnp.abs(den), jnp.exp(-mt))[..., None]
    b_last = b[..., -1]
    g = b_last[..., None] - b + ig
    m_new = jnp.maximum(b_last + m, jnp.max(g, axis=-1))
    ws = jnp.exp(g - m_new[..., None])
    a_last = jnp.exp(b_last + m - m_new)
    C_new = a_last[..., None, None] * C + jnp.einsum('bhsv,bhsk->bhvk', v * ws[..., None], k)
    n_new = a_last[..., None] * n + jnp.einsum('bhs,bhsk->bhk', ws, k)
    return (C_new, n_new, m_new), h


def mlstm(q, k, v, ig, lf, C0, n0, m0):
    B, T, NH, d = q.shape
    Lc = math.gcd(T, ML_CHUNK)
    nc = T // Lc
    f32 = jnp.float32

    def seq_chunks(a):
        return a.astype(f32).reshape(B, nc, Lc, NH, -1).transpose(1, 0, 3, 2, 4)

    def gate_chunks(a):
        return a.astype(f32).reshape(B, nc, Lc, NH).transpose(1, 0, 3, 2)

    xs = (seq_chunks(q), seq_chunks(k) * (d ** -0.5), seq_chunks(v), gate_chunks(ig), gate_chunks(lf))
    (C, n, m), hs = lax.scan(mlstm_chunk, (C0.astype(f32), n0.astype(f32), m0.astype(f32)), xs)
    return hs.transpose(1, 0, 3, 2, 4).reshape(B, T, NH, d), C, n, m


def hier_moe(h, w_grp, b_grp, w_exp, b_exp, w_gate, w_up, w_down):
    B, T, D = h.shape
    f32 = jnp.float32
    t = h.reshape(B * T, D)
    gp = jax.nn.softmax((t @ w_grp + b_grp).astype(f32), axis=-1)
    g_top = jnp.argmax(gp, axis=-1)
    g_w = jnp.max(gp, axis=-1)
    el = (t @ w_exp + b_exp).astype(f32).reshape(-1, N_GROUPS, EXPERTS_PER_GROUP)
    el = jnp.take_along_axis(el, g_top[:, None, None], axis=1)[:, 0]
    e_w, e_idx = lax.top_k(jax.nn.softmax(el, axis=-1), EXPERT_TOPK)
    e_w = e_w / jnp.sum(e_w, axis=-1, keepdims=True)
    inner = jnp.sum(jax.nn.one_hot(e_idx, EXPERTS_PER_GROUP, dtype=f32) * e_w[..., None], axis=1)
    gate = (g_w[:, None, None] * jax.nn.one_hot(g_top, N_GROUPS, dtype=f32)[:, :, None]
            * inner[:, None, :]).reshape(-1, N_EXPERTS)
    hg = jnp.einsum('nd,edf->nef', t, w_gate)
    hu = jnp.einsum('nd,edf->nef', t, w_up)
    act = jax.nn.silu(hg) * hu * gate[..., None].astype(t.dtype)
    return jnp.einsum('nef,efd->nd', act, w_down).reshape(B, T, D)


def decoder_layer(x, c, q_pos, k_pool, v_pool, page_table, C0, n0, m0,
                  w_in, b_ig, b_fg, ml_gain, w_out, g_mix, g_ffn, w_mod, b_mod,
                  w_grp, b_grp, w_exp, b_exp, w_gate, w_up, w_down, slopes):
    B, T, _ = x.shape
    f32 = jnp.float32
    mod = (c @ w_mod + b_mod).reshape(B, N_MOD, 1, D_MODEL)
    shift1, scale1, gate1, shift2, scale2, gate2 = (mod[:, i] for i in range(N_MOD))
    h = rms_norm(x, g_mix) * (1.0 + scale1) + shift1
    cuts = [int(s) for s in np.cumsum([ATT_WIDTH] * 3 + [ML_WIDTH] * 4 + [ML_HEADS])]
    qa, ka, va, qm, km, vm, om, ig, fg = jnp.split(h @ w_in, cuts, axis=-1)
    att_shape = (B, T, ATT_HEADS, ATT_HEAD_DIM)
    ml_shape = (B, T, ML_HEADS, ML_HEAD_DIM)
    qa, ka, va = qa.reshape(att_shape), ka.reshape(att_shape), va.reshape(att_shape)
    if page_table is None:
        kb, vb = to_blocks([ka]), to_blocks([va])
    else:
        past_len = page_table.shape[1] * k_pool.shape[1]
        kb = to_blocks([k_pool[page_table].reshape(B, past_len, ATT_HEADS, ATT_HEAD_DIM), ka])
        vb = to_blocks([v_pool[page_table].reshape(B, past_len, ATT_HEADS, ATT_HEAD_DIM), va])
    att = moba_attention(qa, kb, vb, q_pos, slopes).reshape(B, T, ATT_WIDTH)
    lf = jax.nn.log_sigmoid((fg + b_fg).astype(f32))
    hm, C, n, m = mlstm(qm.reshape(ml_shape), km.reshape(ml_shape), vm.reshape(ml_shape),
                        ig + b_ig, lf, C0, n0, m0)
    hm = hm * lax.rsqrt(jnp.mean(hm * hm, axis=-1, keepdims=True) + EPS) \
        * ml_gain.astype(f32).reshape(ML_HEADS, ML_HEAD_DIM)
    hm = hm.reshape(B, T, ML_WIDTH) * jax.nn.sigmoid(om.astype(f32))
    mixed = jnp.concatenate([att, hm.astype(att.dtype)], axis=-1) @ w_out
    x = x + gate1 * mixed
    h2 = rms_norm(x, g_ffn) * (1.0 + scale2) + shift2
    x = x + gate2 * hier_moe(h2, w_grp, b_grp, w_exp, b_exp, w_gate, w_up, w_down)
    return x, ka, va, C, n, m


def setup_inputs(seed: int = 0) -> dict:
    key = jax.random.key(seed)
    ks = jax.random.split(key, 32)
    f32 = jnp.float32
    n_pages = PAST_LEN // PAGE_SIZE
    n_phys = (DEC_BATCH * n_pages * 5) // 4

    def nrm(k, shape, s):
        return s * jax.random.normal(k, shape, f32)

    dn = D_MODEL ** -0.5
    return {
        'x_prompt': nrm(ks[0], (BATCH, SEQ, D_MODEL), 1.0),
        'x_sample': nrm(ks[1], (DEC_BATCH, DEC_SEQ, D_MODEL), 1.0),
        'cache_k': nrm(ks[2], (DEPTH, n_phys, PAGE_SIZE, ATT_HEADS, ATT_HEAD_DIM), 1.0),
        'cache_v': nrm(ks[3], (DEPTH, n_phys, PAGE_SIZE, ATT_HEADS, ATT_HEAD_DIM), 1.0),
        'page_table': jax.random.permutation(ks[4], n_phys)[: DEC_BATCH * n_pages]
                      .reshape(DEC_BATCH, n_pages).astype(jnp.int32),
        'state_C': nrm(ks[5], (DEPTH, DEC_BATCH, ML_HEADS, ML_HEAD_DIM, ML_HEAD_DIM), 0.3),
        'state_n': nrm(ks[6], (DEPTH, DEC_BATCH, ML_HEADS, ML_HEAD_DIM), 0.3),
        'state_m': nrm(ks[7], (DEPTH, DEC_BATCH, ML_HEADS), 0.5),
        'c_prompt': nrm(ks[8], (BATCH, D_MODEL), 1.0),
        'c_sample': nrm(ks[9], (DEC_BATCH, D_MODEL), 1.0),
        'w_in': nrm(ks[10], (DEPTH, D_MODEL, IN_WIDTH), dn),
        'b_ig': nrm(ks[11], (DEPTH, ML_HEADS), 0.1),
        'b_fg': jnp.linspace(3.0, 6.0, ML_HEADS, dtype=f32)[None, :] + nrm(ks[12], (DEPTH, ML_HEADS), 0.1),
        'ml_gain': 1.0 + nrm(ks[13], (DEPTH, ML_WIDTH), 0.02),
        'w_out': nrm(ks[14], (DEPTH, ATT_WIDTH + ML_WIDTH, D_MODEL), (ATT_WIDTH + ML_WIDTH) ** -0.5),
        'g_mix': 1.0 + nrm(ks[15], (DEPTH, D_MODEL), 0.02),
        'g_ffn': 1.0 + nrm(ks[16], (DEPTH, D_MODEL), 0.02),
        'w_mod': nrm(ks[17], (DEPTH, D_MODEL, N_MOD * D_MODEL), 0.5 * dn),
        'b_mod': nrm(ks[18], (DEPTH, N_MOD * D_MODEL), 0.02),
        'w_grp': nrm(ks[19], (DEPTH, D_MODEL, N_GROUPS), dn),
        'b_grp': nrm(ks[20], (DEPTH, N_GROUPS), 0.01),
        'w_exp': nrm(ks[21], (DEPTH, D_MODEL, N_EXPERTS), dn),
        'b_exp': nrm(ks[22], (DEPTH, N_EXPERTS), 0.01),
        'w_gate': nrm(ks[23], (DEPTH, N_EXPERTS, D_MODEL, EXPERT_FF), dn),
        'w_up': nrm(ks[24], (DEPTH, N_EXPERTS, D_MODEL, EXPERT_FF), dn),
        'w_down': nrm(ks[25], (DEPTH, N_EXPERTS, EXPERT_FF, D_MODEL), EXPERT_FF ** -0.5),
        'g_final': 1.0 + nrm(ks[26], (D_MODEL,), 0.02),
    }


def reference(x_prompt, x_sample, cache_k, cache_v, page_table, state_C, state_n, state_m,
              c_prompt, c_sample, w_in, b_ig, b_fg, ml_gain, w_out, g_mix, g_ffn, w_mod, b_mod,
              w_grp, b_grp, w_exp, b_exp, w_gate, w_up, w_down, g_final):
    f32 = jnp.float32
    slopes = alibi_slopes()
    Bp, Tp, _ = x_prompt.shape
    Bd, Td, _ = x_sample.shape
    past_len = page_table.shape[1] * cache_k.shape[2]
    pos_p = jnp.arange(Tp, dtype=jnp.int32)
    pos_d = past_len + jnp.arange(Td, dtype=jnp.int32)
    C0p = jnp.zeros((Bp, ML_HEADS, ML_HEAD_DIM, ML_HEAD_DIM), f32)
    n0p = jnp.zeros((Bp, ML_HEADS, ML_HEAD_DIM), f32)
    m0p = jnp.zeros((Bp, ML_HEADS), f32)
    xp, xd = x_prompt, x_sample
    kps, vps, kds, vds = [], [], [], []
    Cps, nps, mps, Cds, nds, mds = [], [], [], [], [], []
    for l in range(DEPTH):
        w = (w_in[l], b_ig[l], b_fg[l], ml_gain[l], w_out[l], g_mix[l], g_ffn[l], w_mod[l], b_mod[l],
             w_grp[l], b_grp[l], w_exp[l], b_exp[l], w_gate[l], w_up[l], w_down[l])
        xp, kp, vp, Cp, n_p, mp = decoder_layer(xp, c_prompt, pos_p, None, None, None,
                                                C0p, n0p, m0p, *w, slopes)
        xd, kd, vd, Cd, n_d, md = decoder_layer(xd, c_sample, pos_d, cache_k[l], cache_v[l], page_table,
                                                state_C[l], state_n[l], state_m[l], *w, slopes)
        kps.append(kp); vps.append(vp); kds.append(kd); vds.append(vd)
        Cps.append(Cp); nps.append(n_p); mps.append(mp)
        Cds.append(Cd); nds.append(n_d); mds.append(md)
    y_prompt = rms_norm(xp, g_final)
    y_sample = rms_norm(xd, g_final)
    return (y_prompt, y_sample, jnp.stack(kps), jnp.stack(vps), jnp.stack(kds), jnp.stack(vds),
            jnp.stack(Cps), jnp.stack(nps), jnp.stack(mps), jnp.stack(Cds), jnp.stack(nds), jnp.stack(mds))
```

```python
import contextlib
import os
import numpy as np
import ml_dtypes
import concourse.bass as bass
import concourse.mybir as mybir
from concourse.bass_utils import run_bass_kernel_spmd

F32 = mybir.dt.float32
BF16 = mybir.dt.bfloat16
I32 = mybir.dt.int32
ACT = mybir.ActivationFunctionType
ALU = mybir.AluOpType
AX = mybir.AxisListType

D = 2048
KT = 16
NOWN = 1024
NPRE = 1024
NSMP = 32
NALL = NPRE + NOWN + NSMP
NMY = NOWN + NSMP
IN_W = 7176
EPS = 1e-6
BIG = 1.0e30
SCALE_A = 128 ** -0.5
ENGS = ("tensor", "vector", "scalar", "gpsimd", "sync")
ROT = 30000


class Prog:
    def __init__(self, nc, stack):
        self.nc = nc
        self.stack = stack
        self.q = {e: [] for e in ENGS}
        self.esem = {}
        self.ecnt = {}
        self.nsem = 0
        for e in ENGS:
            self._new_esem(e)
        self.waited = {e: {} for e in ENGS}
        self.last_w = {}
        self.readers = {}
        self.dsem = {}
        self.out_tokens = []
        self.bank_last = {}
        self.all_dsems = []
        self.barrier_toks = []
        self.barrier_seen = {e: 0 for e in ENGS}
        self.barrier_id = 0

    def barrier(self):
        toks = []
        for e in ENGS:
            if self.ecnt[e] > 0:
                toks.append((self.esem[e], self.ecnt[e], "bar"))
        for ent in self.all_dsems:
            if ent[1] > 0:
                toks.append((ent[0], ent[1], "bar"))
        self.barrier_toks = toks
        self.barrier_id += 1

    def _bar(self, eng):
        if self.barrier_seen[eng] < self.barrier_id:
            self.barrier_seen[eng] = self.barrier_id
            return list(self.barrier_toks)
        return []

    def _banks(self, reads, writes):
        return {k[:2] for k in list(reads) + list(writes) if isinstance(k, tuple) and k and k[0] in ("ps", "psb")}

    def _sem(self, name):
        s = self.stack.enter_context(self.nc.semaphore(f"{name}_{self.nsem}"))
        self.nsem += 1
        return s

    def _new_esem(self, e):
        self.esem[e] = self._sem("e" + e[:2])
        self.ecnt[e] = 0

    def _waits(self, eng, deps):
        need = {}
        for t in deps:
            if t is None:
                continue
            s, v, src = t
            if src == eng and eng == "tensor":
                continue
            sid = id(s)
            if self.waited[eng].get(sid, 0) >= v:
                continue
            if need.get(sid, (None, 0))[1] < v:
                need[sid] = (s, v)
        out = []
        for sid, (s, v) in need.items():
            self.waited[eng][sid] = v
            out.append((s, v))
        return out

    def _deps(self, reads, writes):
        deps = []
        for k in reads:
            deps.append(self.last_w.get(k))
        for k in writes:
            deps.append(self.last_w.get(k))
            deps.extend(self.readers.get(k, ()))
        return deps

    def _commit(self, tok, reads, writes):
        for k in reads:
            self.readers.setdefault(k, []).append(tok)
        for k in writes:
            self.last_w[k] = tok
            self.readers[k] = []

    def op(self, eng, fn, reads=(), writes=()):
        deps = self._deps(reads, writes) + self._bar(eng)
        banks = self._banks(reads, writes)
        for b in banks:
            for e2, t2 in self.bank_last.get(b, {}).items():
                if e2 != eng:
                    deps.append(t2)
        waits = self._waits(eng, deps)
        if self.ecnt[eng] >= ROT:
            self._new_esem(eng)
        self.ecnt[eng] += 1
        s, v = self.esem[eng], self.ecnt[eng]
        self.q[eng].append((waits, fn, s, 1))
        tok = (s, v, eng)
        self._commit(tok, reads, writes)
        for b in banks:
            self.bank_last.setdefault(b, {})[eng] = tok
        return tok

    def dma(self, eng, fn, reads=(), writes=(), key=None, is_out=False):
        waits = self._waits(eng, self._deps(reads, writes) + self._bar(eng))
        if key is None:
            key = writes[0] if writes else reads[0]
        ent = self.dsem.get(key)
        if ent is None or ent[1] >= 16 * 1500:
            ent = [self._sem("d"), 0]
            self.dsem[key] = ent
            self.all_dsems.append(ent)
        ent[1] += 16
        self.q[eng].append((waits, fn, ent[0], 16))
        tok = (ent[0], ent[1], "dma")
        self._commit(tok, reads, writes)
        if is_out:
            self.out_tokens.append(tok)
        return tok

    def unify(self, key, wkeys):
        ent = self.dsem[key]
        tok = (ent[0], ent[1], "dma")
        for k in wkeys:
            self.last_w[k] = tok

    def finish(self):
        last = {}
        for s, v, _ in self.out_tokens:
            if last.get(id(s), (None, 0))[1] < v:
                last[id(s)] = (s, v)
        fin = list(last.values())
        q = self.q
        with self.nc.Block() as block:
            def mk(ename):
                def body(eng):
                    for waits, fn, s, inc in q[ename]:
                        for ws, wv in waits:
                            eng.wait_ge(ws, wv)
                        fn(eng).then_inc(s, inc)
                    if ename == "sync":
                        for s, v in fin:
                            eng.wait_ge(s, v)
                return body
            block.tensor(mk("tensor"))
            block.vector(mk("vector"))
            block.scalar(mk("scalar"))
            block.gpsimd(mk("gpsimd"))
            block.sync(mk("sync"))


class Ctx:
    def __init__(self, nc, stack):
        self.nc = nc
        self.stack = stack
        self.P = Prog(nc, stack)
        self.ins = {}
        self.outs = {}
        self.nwb = 0

    def inp(self, name, shape, dt=F32):
        ap = self.nc.dram_tensor(name, list(shape), dt, kind="ExternalInput").ap()
        self.ins[name] = ap
        return ap

    def outp(self, name, shape, dt=F32):
        ap = self.nc.dram_tensor(name, list(shape), dt, kind="ExternalOutput").ap()
        self.outs[name] = ap
        return ap

    def sb(self, name, shape, dt=F32, stack=None):
        return (stack or self.stack).enter_context(self.nc.sbuf_tensor(name, list(shape), dt))

    def psum(self, name, shape, dt=F32):
        return self.stack.enter_context(self.nc.psum_tensor(name, list(shape), dt))

    def mm(self, out, lhsT, rhs, start, stop, r, w):
        return self.P.op("tensor", lambda e: e.matmul(out, lhsT, rhs, start=start, stop=stop), r, w)

    def tr(self, out, in_, ident, r, w):
        return self.P.op("tensor", lambda e: e.transpose(out, in_, ident), r, w)

    def act(self, out, in_, func, r, w, bias=None, scale=None):
        kw = {}
        if bias is not None:
            kw["bias"] = bias
        if scale is not None:
            kw["scale"] = scale
        return self.P.op("scalar", lambda e: e.activation(out, in_, func, **kw), r, w)

    def tt(self, out, in0, in1, op, r, w, eng="vector"):
        return self.P.op(eng, lambda e: e.tensor_tensor(out, in0, in1, op), r, w)

    def ts(self, out, in0, s1, op0, r, w, s2=None, op1=None, eng="vector"):
        if op1 is None:
            return self.P.op(eng, lambda e: e.tensor_scalar(out, in0, s1, None, op0), r, w)
        return self.P.op(eng, lambda e: e.tensor_scalar(out, in0, s1, s2, op0, op1), r, w)

    def stt(self, out, in0, scalar, in1, op0, op1, r, w):
        return self.P.op("vector", lambda e: e.scalar_tensor_tensor(out, in0, scalar, in1, op0, op1), r, w)

    def red(self, out, in_, op, r, w, axis=AX.X):
        return self.P.op("vector", lambda e: e.tensor_reduce(out, in_, axis, op), r, w)

    def cp(self, out, in_, r, w, eng="vector"):
        if eng == "scalar":
            return self.P.op("scalar", lambda e: e.activation(out, in_, ACT.Identity), r, w)
        if eng == "gpsimd":
            return self.P.op("gpsimd", lambda e: e.tensor_copy(out, in_), r, w)
        return self.P.op(eng, lambda e: e.tensor_scalar(out, in_, 1.0, None, ALU.mult), r, w)

    def recip(self, out, in_, r, w):
        return self.P.op("vector", lambda e: e.reciprocal(out, in_), r, w)

    def memset(self, ap, val, w, eng="vector"):
        return self.P.op(eng, lambda e: e.memset(ap, val), (), w)

    def ld(self, out, in_, w, r=(), eng="sync", key=None, slow=False):
        if slow:
            return self.P.dma(eng, lambda e: e.dma_start(out=out, in_=in_, allow_slow_non_contiguous=True), r, w, key=key)
        return self.P.dma(eng, lambda e: e.dma_start(out=out, in_=in_), r, w, key=key)

    def st(self, out, in_, r, w, eng="sync", key=None, slow=False):
        if slow:
            return self.P.dma(eng, lambda e: e.dma_start(out=out, in_=in_, allow_slow_non_contiguous=True), r, w, key=key, is_out=True)
        return self.P.dma(eng, lambda e: e.dma_start(out=out, in_=in_), r, w, key=key, is_out=True)

    def alloc_wbufs(self, n):
        self.wbufs = [self.sb(f"wbuf{i}", [128, KT, 512], BF16) for i in range(n)]
        self.wnext = 0

    def load_w(self, src3):
        i = self.wnext % len(self.wbufs)
        self.wnext += 1
        buf = self.wbufs[i]
        nk, ncol = src3.shape[1], src3.shape[2]
        flat = buf[:].rearrange("p a b -> p (a b)")
        view = flat.rearrange("p (j d) -> p j d", j=nk) if (nk, ncol) != (KT, 512) else buf[:]
        key = ("W", i)
        nq = 4
        step = nk // nq
        for qd in range(nq):
            self.P.dma("gpsimd",
                       (lambda e, o=view[:, qd * step:(qd + 1) * step, :], s=src3[:, qd * step:(qd + 1) * step, :]:
                        e.dma_start(out=o, in_=s)),
                       reads=(), writes=[("W", i, qd), key] if qd == 0 else [("W", i, qd)],
                       key=("Wd", i, qd))
        return view, [("W", i, qd) for qd in range(nq)] + [key]


def _wkeys_write_guard(C, wk):
    return wk


SUB = [99]


def build_main(stage=99, dbg=()):
    nc = bass.Bass("TRN2", target_bir_lowering=False)
    stack = contextlib.ExitStack()
    with stack:
        C = Ctx(nc, stack)
        P = C.P
        xo = C.inp("xo", [D, NOWN]); xp = C.inp("xp", [D, NPRE]); xs = C.inp("xs", [D, NSMP])
        cT = C.inp("cT", [D, 5])
        wmod = C.inp("wmod", [D, 6 * D]); bmodT = C.inp("bmodT", [128, 96])
        gvec = C.inp("gvec", [128, 48])
        win = C.inp("win", [D, IN_W]); wout = C.inp("wout", [D, D]); wr = C.inp("wr", [D, 20])
        rowc = C.inp("rowc", [128, 28 + 1024])
        wg = C.inp("wg", [16, D, 512]); wu = C.inp("wu", [16, D, 512]); wd = C.inp("wd", [16, 512, D])
        atts = C.inp("atts", [1024, NSMP])
        sC = C.inp("sC", [4, 4, 256, 256]); sn = C.inp("sn", [4, 4, 256]); sm = C.inp("sm", [128, 16])
        flags = C.inp("flags", [128, 8])
        c_ident = C.inp("c_ident", [128, 128]); c_U = C.inp("c_U", [128, 64]); c_neg = C.inp("c_neg", [128, 64])
        c_cm = C.inp("c_cm", [128, 512]); c_abase = C.inp("c_abase", [128, 128]); c_gb = C.inp("c_gb", [128, 32])
        c_aq = C.inp("c_aq", [8, 3, 256]); c_ind = C.inp("c_ind", [128, 8 * 128]); c_sel = C.inp("c_sel", [16, 16 * 128])

        yo = C.outp("yo", [D, NOWN]); ys = C.outp("ys", [D, NSMP])
        ko = C.outp("ko", [1024, NOWN]); vo = C.outp("vo", [NOWN, 1024])
        Cp = C.outp("Cp", [4, 256, 256]); npo = C.outp("npo", [4, 256]); mpo = C.outp("mpo", [1, 4])
        Cs = C.outp("Cs", [4, 4, 256, 256]); nso = C.outp("nso", [4, 4, 256]); mso = C.outp("mso", [1, 16])
        dbg_out = {}

        C.alloc_wbufs(3)
        ident = C.sb("ident", [128, 128]); identb = C.sb("identb", [128, 128], BF16)
        onesb = C.sb("onesb", [128, 128], BF16); onesf = C.sb("onesf", [128, 128])
        Utri = C.sb("Utri", [128, 64]); negm = C.sb("negm", [128, 64])
        cm = C.sb("cm", [128, 512], BF16); abase = C.sb("abase", [128, 128]); gb = C.sb("gb", [128, 32])
        indT = C.sb("indT", [128, 8 * 128], BF16)
        gv = C.sb("gv", [128, 48]); bmod = C.sb("bmod", [128, 96]); rc = C.sb("rc", [128, 28 + 1024])
        flg = C.sb("flg", [128, 8]); smt = C.sb("smt", [128, 16])
        modT = C.sb("modT", [128, 96, 5]); A1 = C.sb("A1", [128, 16, 5]); A2 = C.sb("A2", [128, 16, 5])
        cTb = C.sb("cTb", [128, KT, 5], BF16)
        wgate = C.sb("wgate", [128, KT, 8], BF16); wrb = C.sb("wrb", [128, KT, 20], BF16)
        R1 = C.sb("R1", [128, 67584 // 2], BF16)
        hT = R1[:, 0:KT * NALL].rearrange("p (k n) -> p k n", k=KT)
        mixT = C.sb("mixT", [128, KT, NMY], BF16)
        ps = [C.psum(f"ps{i}", [128, 512]) for i in range(7)]
        psb = C.psum("psb", [128, 1024], BF16)

        ck = []
        for (dst, src, k) in [(ident, c_ident, "ident"), (Utri, c_U, "Utri"), (negm, c_neg, "negm"),
                              (abase, c_abase, "abase"), (gb, c_gb, "gb"), (gv, gvec, "gv"), (bmod, bmodT, "bmod"),
                              (rc, rowc, "rc"), (flg, flags, "flg"), (smt, sm, "smt")]:
            C.ld(dst[:], src, [k], key=("constS",)); ck.append(k)
        P.unify(("constS",), ck)
        ck = []
        for (dst, src, k) in [(identb[:], c_ident, "identb"), (cm[:], c_cm, "cm"), (indT[:], c_ind, "indT"),
                              (cTb[:], cT.rearrange("(k p) r -> p k r", p=128), "cTb"),
                              (wgate[:], win[:, 7168:7176].rearrange("(k p) c -> p k c", p=128), "wgate"),
                              (wrb[:], wr.rearrange("(k p) c -> p k c", p=128), "wrb")]:
            C.ld(dst, src, [k], eng="gpsimd", key=("constG",)); ck.append(k)
        P.unify(("constG",), ck)
        C.memset(onesb[:], 1.0, ["onesb"]); C.memset(onesf[:], 1.0, ["onesf"])

        wmv = wmod.rearrange("(k p) c -> p k c", p=128)
        for g in range(24):
            W, wk = C.load_w(wmv[:, :, g * 512:(g + 1) * 512])
            pb = ps[g % 2]
            for j in range(4):
                for kt in range(KT):
                    C.mm(pb[:, j * 8:j * 8 + 5], W[:, kt, j * 128:(j + 1) * 128], cTb[:, kt, :], kt == 0, kt == KT - 1,
                         wk + ["cTb"], [("ps", g % 2)])
            for j in range(4):
                ft = g * 4 + j
                C.ts(modT[:, ft, :], pb[:, j * 8:j * 8 + 5], bmod[:, ft:ft + 1], ALU.add,
                     [("ps", g % 2), "bmod"], ["modT"])
        for r in range(5):
            C.stt(A1[:, :, r], modT[:, 16:32, r], 1.0, gv[:, 0:16], ALU.add, ALU.mult, ["modT", "gv"], ["A1"])
            C.stt(A2[:, :, r], modT[:, 64:80, r], 1.0, gv[:, 16:32], ALU.add, ALU.mult, ["modT", "gv"], ["A2"])
        if "modT" in dbg:
            o = C.outp("d_modT", [128, 96 * 5]); C.st(o, modT[:].rearrange("p a b -> p (a b)"), ["modT"], ["d_modT"])

        EPS_AP = C.sb("epsap", [128, 1])
        C.memset(EPS_AP[:], EPS, ["eps"])
        groupsB = [(xp[:, 0:512], 0, 512, [(0, 512, 0)]), (xp[:, 512:1024], 512, 512, [(0, 512, 0)]),
                   (xo[:, 0:512], 1024, 512, [(0, 512, 0)]), (xo[:, 512:1024], 1536, 512, [(0, 512, 0)]),
                   (xs, 2048, 32, [(8 * j, 8, 1 + j) for j in range(4)])]
        with contextlib.ExitStack() as st2:
            xt = [C.sb(f"B_xt{i}", [128, 512], stack=st2) for i in range(3)]
            sq = [C.sb(f"B_sq{i}", [128, 512], BF16, stack=st2) for i in range(2)]
            rs = [C.sb(f"B_rs{i}", [128, 512], stack=st2) for i in range(2)]
            tm = [C.sb(f"B_tm{i}", [128, 512], stack=st2) for i in range(2)]
            cnt = 0
            for gi, (src, col0, n, rows) in enumerate(groupsB):
                pacc = ps[2 + gi % 2]
                pk = ("ps", 2 + gi % 2)
                for ft in range(KT):
                    b = cnt % 3; cnt += 1
                    C.ld(xt[b][:, 0:n], src[ft * 128:(ft + 1) * 128, :], [("Bxt", b)])
                    C.act(sq[ft % 2][:, 0:n], xt[b][:, 0:n], ACT.Square, [("Bxt", b)], [("Bsq", ft % 2)])
                    C.mm(pacc[:, 0:n], onesb[:], sq[ft % 2][:, 0:n], ft == 0, ft == KT - 1, [("Bsq", ft % 2), "onesb"], [pk])
                rsd = rs[gi % 2]
                rk = ("Brs", gi % 2)
                C.act(rsd[:, 0:n], pacc[:, 0:n], ACT.Sqrt, [pk, "eps"], [rk], bias=EPS_AP[:, 0:1], scale=1.0 / D)
                C.recip(rsd[:, 0:n], rsd[:, 0:n], [rk], [rk])
                for ft in range(KT):
                    b = cnt % 3; cnt += 1
                    C.ld(xt[b][:, 0:n], src[ft * 128:(ft + 1) * 128, :], [("Bxt", b)])
                    t = tm[ft % 2]
                    C.tt(t[:, 0:n], xt[b][:, 0:n], rsd[:, 0:n], ALU.mult, [("Bxt", b), rk], [("Btm", ft % 2)])
                    for (c0, cn, r) in rows:
                        C.act(hT[:, ft, col0 + c0:col0 + c0 + cn], t[:, c0:c0 + cn], ACT.Identity,
                              [("Btm", ft % 2), "A1", "modT"], [("Bdst", gi)],
                              bias=modT[:, ft, r:r + 1], scale=A1[:, ft, r:r + 1])
        P.barrier()
        HK = [("Bdst", gi) for gi in range(5)]

        def hkeys(c0, n):
            ks = []
            for gi, (_, col0, gn, _) in enumerate(groupsB):
                if c0 < col0 + gn and c0 + n > col0:
                    ks.append(("Bdst", gi))
            return ks

        if "hT" in dbg:
            o = C.outp("d_hT", [128, KT * NALL])
            C.ld(o, R1[:, 0:KT * NALL], ["d_hT"], r=HK, eng="gpsimd")
            P.out_tokens.append(P.last_w["d_hT"])

        if stage >= 2:
            _phase_attention(C, ps, psb, hT, hkeys, mixT, win, ko, vo, ident, identb, onesb, cm, abase, gb, indT,
                             c_aq, dbg)
        P.barrier()
        if stage >= 3:
            _phase_mlstm(C, ps, psb, hT, hkeys, mixT, win, wgate, rc, flg, smt, sC, sn, Cp, npo, mpo, Cs, nso, mso,
                         ident, identb, onesf, Utri, negm, dbg)
        if "mixA" in dbg:
            o = C.outp("d_mixA", [128, 8, 1024])
            C.ld(o, mixT[:, 0:8, 0:1024], ["d_mixA"], r=[("mix", f) for f in range(8)], eng="gpsimd")
            P.out_tokens.append(P.last_w["d_mixA"])
        P.barrier()
        if "mixT" in dbg:
            o = C.outp("d_mixT", [128, KT * NMY])
            C.ld(o, mixT[:].rearrange("p a b -> p (a b)"), ["d_mixT"], r=[("mix", f) for f in range(16)], eng="gpsimd")
            P.out_tokens.append(P.last_w["d_mixT"])
        if stage >= 4:
            _phase_tail(C, ps, R1, mixT, xo, xs, atts, wout, wg, wu, wd, wrb, rc, gv, modT, A2, onesb, onesf, ident,
                        c_sel, EPS_AP, yo, ys, dbg)
        P.finish()
    return nc


def _phase_attention(C, ps, psb, hT, hkeys, mixT, win, ko, vo, ident, identb, onesb, cm, abase, gb, indT, c_aq, dbg):
    P = C.P
    winv = win.rearrange("(k p) c -> p k c", p=128)
    import os
    if os.environ.get("HACK") == "1":
        winv = C.ins["wmod"].rearrange("(k p) c -> p k c", p=128)
    with contextlib.ExitStack() as st2:
        KTb = C.sb("KTb", [128, 2048], BF16, stack=st2)
        Vb = C.sb("Vb", [128, 16, 512], BF16, stack=st2)
        qf = C.sb("qf", [128, NOWN], stack=st2)
        qb = C.sb("qb", [128, NOWN], BF16, stack=st2)
        kst = [C.sb(f"kst{i}", [128, 512], stack=st2) for i in range(2)]
        vst = [C.sb(f"vst{i}", [128, 512], stack=st2) for i in range(2)]
        sqt = [C.sb(f"sqt{i}", [128, 512], BF16, stack=st2) for i in range(2)]
        kmT = C.sb("kmT", [128, 8], stack=st2)
        nmax = C.sb("nmax", [128, 4], stack=st2)
        abias = C.sb("abias", [128, 16], stack=st2)
        Gp = C.sb("Gp", [128, 2, 8], stack=st2)
        top8 = C.sb("top8", [128, 2, 8], stack=st2)
        bbt = C.sb("bbt", [128, 2, 8], stack=st2)
        aug = [C.sb(f"aug{i}", [128, 256], BF16, stack=st2) for i in range(2)]
        for i in range(2):
            C.memset(aug[i][:], 0.0, [("aug", i), ("aug", i, "q")])
        PT = [C.sb(f"PT{i}", [128, 256], BF16, stack=st2) for i in range(4)]
        rden = C.sb("rden", [128, 256], stack=st2)

        sqc = 0
        for hg in range(2):
            Wq, wqk = C.load_w(winv[:, :, hg * 512:(hg + 1) * 512])
            Wk, wkk = C.load_w(winv[:, :, 1024 + hg * 512:1024 + (hg + 1) * 512])
            Wv, wvk = C.load_w(winv[:, :, 2048 + hg * 512:2048 + (hg + 1) * 512])
            for tt_ in range(16):
                pb = ps[tt_ % 2]; pk = ("ps", tt_ % 2)
                for kt in range(KT):
                    C.mm(pb[:, :], hT[:, kt, tt_ * 128:(tt_ + 1) * 128], Wv[:, kt, :], kt == 0, kt == KT - 1,
                         wvk + hkeys(tt_ * 128, 128), [pk])
                C.cp(Vb[:, tt_, :], pb[:, :], [pk], [("Vb", tt_)], eng="scalar")
                if tt_ >= 8 and os.environ.get('NOVST') != '1':
                    s = vst[tt_ % 2]
                    C.cp(s[:, :], pb[:, :], [pk], [("vst", tt_ % 2)])
                    if os.environ.get('NOVST') != '2':
                        C.st(vo[(tt_ - 8) * 128:(tt_ - 7) * 128, hg * 512:(hg + 1) * 512], s[:, :], [("vst", tt_ % 2)],
                             [("vo", hg, tt_)], key=("vst", tt_ % 2))
            if SUB[0] < 1:
                continue
            for hl in range(4):
                h = hg * 4 + hl
                C.memset(nmax[:, 0:2], 0.0, ["nmax"])
                for tg in range(4):
                    pb = ps[tg % 2]; pk = ("ps", tg % 2)
                    for kt in range(KT):
                        C.mm(pb[:, :], Wk[:, kt, hl * 128:(hl + 1) * 128], hT[:, kt, tg * 512:(tg + 1) * 512], kt == 0,
                             kt == KT - 1, wkk + hkeys(tg * 512, 512), [pk])
                    C.cp(KTb[:, tg * 512:(tg + 1) * 512], pb[:, :], [pk], [("KTb", tg)], eng="scalar")
                    C.red(kmT[:, tg * 2:tg * 2 + 2], pb[:, :].rearrange("p (a b) -> p a b", a=2), ALU.add, [pk], ["kmT"])
                    sq = sqt[sqc % 2]
                    C.act(sq[:, :], pb[:, :], ACT.Square, [pk], [("sqt", sqc % 2)])
                    pn = ps[2]
                    C.mm(pn[:, :], onesb[:], sq[:, :], True, True, [("sqt", sqc % 2), "onesb"], [("ps", 2), ("ps", 2, "t")])
                    sqc += 1
                    C.red(nmax[:, 3:4], pn[:, :], ALU.max, [("ps", 2)], ["nmax3"])
                    C.tt(nmax[:, 0:1], nmax[:, 0:1], nmax[:, 3:4], ALU.max, ["nmax3", "nmax"], ["nmax"])
                    if tg >= 2:
                        s = kst[tg % 2]
                        C.cp(s[:, :], pb[:, :], [pk], [("kst", tg % 2)])
                        C.st(ko[h * 128:(h + 1) * 128, (tg - 2) * 512:(tg - 1) * 512], s[:, :], [("kst", tg % 2)],
                             [("ko", h, tg)], key=("kst", tg % 2))
                C.ts(kmT[:, :], kmT[:, :], 1.0 / 256.0, ALU.mult, ["kmT"], ["kmT"])
                for tg in range(2):
                    pb = ps[tg % 2]; pk = ("ps", tg % 2)
                    for kt in range(KT):
                        C.mm(pb[:, :], Wq[:, kt, hl * 128:(hl + 1) * 128], hT[:, kt, 1024 + tg * 512:1024 + (tg + 1) * 512],
                             kt == 0, kt == KT - 1, wqk + hkeys(1024 + tg * 512, 512), [pk])
                    C.cp(qf[:, tg * 512:(tg + 1) * 512], pb[:, :], [pk], [("qf", tg)])
                    C.act(qb[:, tg * 512:(tg + 1) * 512], pb[:, :], ACT.Identity, [pk], [("qb", tg)], scale=SCALE_A)
                    sq = sqt[sqc % 2]
                    C.act(sq[:, :], pb[:, :], ACT.Square, [pk], [("sqt", sqc % 2)])
                    pn = ps[2]
                    C.mm(pn[:, :], onesb[:], sq[:, :], True, True, [("sqt", sqc % 2), "onesb"], [("ps", 2), ("ps", 2, "t")])
                    sqc += 1
                    C.red(nmax[:, 3:4], pn[:, :], ALU.max, [("ps", 2)], ["nmax3"])
                    C.tt(nmax[:, 1:2], nmax[:, 1:2], nmax[:, 3:4], ALU.max, ["nmax3", "nmax"], ["nmax"])
                C.tt(nmax[:, 2:3], nmax[:, 0:1], nmax[:, 1:2], ALU.mult, ["nmax"], ["nmaxc"])
                C.act(nmax[:, 2:3], nmax[:, 2:3], ACT.Sqrt, ["nmaxc"], ["nmaxc"], scale=SCALE_A * SCALE_A)
                C.ts(abias[:, :], abase[:, h * 16:(h + 1) * 16], nmax[:, 2:3], ALU.subtract, ["abase", "nmaxc"], ["abias"])
                if SUB[0] < 2:
                    continue
                ag = aug[h % 2]
                C.ld(ag[8:11, :], c_aq[h], [("aug", h % 2, "q")], r=[("aug", h % 2)], eng="gpsimd")
                for qg in range(4):
                    c = 4 + qg
                    pg = ps[2]
                    for qt in range(2):
                        q0 = qg * 256 + qt * 128
                        C.mm(pg[:, qt * 8:(qt + 1) * 8], qf[:, q0:q0 + 128], kmT[:, :], True, True,
                             [("qf", q0 // 512), "kmT"], [("ps", 2)])
                    for qt in range(2):
                        C.tt(Gp[:, qt, :], pg[:, qt * 8:(qt + 1) * 8], gb[:, qg * 8:(qg + 1) * 8], ALU.add,
                             [("ps", 2), "gb"], ["Gp"])
                        P.op("vector", lambda e, o=top8[:, qt, :], i=Gp[:, qt, :]: e.max(o, i), ["Gp"], ["top8"])
                        C.ts(bbt[:, qt, :], Gp[:, qt, :], top8[:, qt, 2:3], ALU.is_ge, ["Gp", "top8"], ["bbt"])
                        C.ts(bbt[:, qt, :], bbt[:, qt, :], BIG, ALU.mult, ["bbt"], ["bbt"], s2=-BIG, op1=ALU.add)
                        C.tt(bbt[:, qt, :], bbt[:, qt, :], gb[:, qg * 8:(qg + 1) * 8], ALU.add, ["bbt", "gb"], ["bbt"])
                        C.memset(bbt[:, qt, c:c + 1], 0.0, ["bbt"])
                    pt_ = ps[2]
                    for qt in range(2):
                        C.tr(pt_[0:8, 256 + qt * 128:256 + (qt + 1) * 128], bbt[:, qt, :], ident[:], ["bbt", "ident"],
                             [("ps", 2, "t")])
                    C.cp(ag[0:8, :], pt_[0:8, 256:512], [("ps", 2, "t")], [("aug", h % 2)], eng="scalar")
                    if SUB[0] < 3:
                        continue
                    po = ps[5]; pd = ps[6]
                    nkt = 2 * (c + 1)
                    for kt_ in range(nkt):
                        n = kt_ // 2
                        slot = kt_ % 4
                        bank = (0, 1, 3, 4)[kt_ % 4]
                        pss = ps[bank][:, 0:256]
                        skey = ("ps", bank)
                        C.mm(pss, KTb[:, kt_ * 128:(kt_ + 1) * 128], qb[:, qg * 256:(qg + 1) * 256], True, False,
                             [("KTb", kt_ // 4), ("qb", qg // 2)], [skey])
                        diag = (n == c)
                        C.mm(pss, indT[:, n * 128:(n + 1) * 128], ag[:, :], False, not diag,
                             ["indT", ("aug", h % 2), ("aug", h % 2, "q")], [skey])
                        if diag:
                            ktl = kt_ - 2 * c
                            C.mm(pss, identb[:], cm[:, ktl * 256:(ktl + 1) * 256], False, True, ["identb", "cm"], [skey])
                        pt = PT[kt_ % 4]
                        di = kt_ - 2 * c + 14
                        C.act(pt[:, :], pss, ACT.Exp, [skey, "abias"], [("PT", kt_ % 4)], bias=abias[:, di:di + 1], scale=1.0)
                        C.mm(po[:, 0:256], Vb[:, kt_, hl * 128:(hl + 1) * 128], pt[:, :], kt_ == 0, kt_ == nkt - 1,
                             [("Vb", kt_), ("PT", kt_ % 4)], [("ps", 5)])
                        C.mm(pd[:, 0:256], onesb[:], pt[:, :], kt_ == 0, kt_ == nkt - 1,
                             ["onesb", ("PT", kt_ % 4)], [("ps", 6)])
                    C.recip(rden[:, :], pd[:, 0:256], [("ps", 6)], ["rden"])
                    C.tt(mixT[:, h, qg * 256:(qg + 1) * 256], po[:, 0:256], rden[:, :], ALU.mult, [("ps", 5), "rden"],
                         [("mix", h)])


def _phase_mlstm(C, ps, psb, hT, hkeys, mixT, win, wgate, rc, flg, smt, sC, sn, Cp, npo, mpo, Cs, nso, mso,
                 ident, identb, onesf, Utri, negm, dbg):
    P = C.P
    winv = win.rearrange("(k p) c -> p k c", p=128)
    with contextlib.ExitStack() as st2:
        qmT = C.sb("qmT", [128, 4, NMY], BF16, stack=st2)
        kmT = C.sb("kmTm", [128, 4, NALL], BF16, stack=st2)
        S = C.sb("S", [128, 4, 256], stack=st2)
        Sb = C.sb("Sb", [128, 4, 256], BF16, stack=st2)
        nS = C.sb("nS", [128, 4], stack=st2); nSb = C.sb("nSb", [128, 4], BF16, stack=st2)
        mrep = C.sb("mrep", [128, 2], stack=st2); mnew = C.sb("mnew", [128, 2], stack=st2)
        gts = C.sb("gts", [64, 8], stack=st2); lf = C.sb("lf", [64, 4], stack=st2)
        bsb = C.sb("bsb", [64, 4], stack=st2); bl = C.sb("bl", [128, 4], stack=st2)
        u = C.sb("u", [64, 4], stack=st2); Dg = C.sb("Dg", [64, 2, 64], stack=st2)
        mmax = C.sb("mmax", [128, 2], stack=st2); t2 = C.sb("t2", [128, 2], stack=st2)
        wsv = C.sb("wsv", [64, 2], stack=st2); wsb = C.sb("wsb", [64, 2], BF16, stack=st2)
        alast = C.sb("alast", [128, 2], stack=st2)
        ktok = C.sb("ktok", [64, 512], BF16, stack=st2); vws = C.sb("vws", [64, 2, 256], BF16, stack=st2)
        vbf = C.sb("vbf", [64, 2, 256], BF16, stack=st2)
        dm = C.sb("dm", [64, 2, 64], stack=st2); rowmax = C.sb("rowmax", [64, 2], stack=st2)
        mt = C.sb("mt", [64, 2], stack=st2); t3 = C.sb("t3", [64, 2], stack=st2)
        wmat = C.sb("wmat", [64, 2, 64], stack=st2); av = C.sb("av", [64, 2], stack=st2); em = C.sb("em", [64, 2], stack=st2)
        sc = C.sb("sc", [64, 2, 64], stack=st2); dsum = C.sb("dsum", [64, 2], stack=st2)
        scT = C.sb("scT", [64, 2, 64], BF16, stack=st2)
        tnum = C.sb("tnum", [64, 256], stack=st2); num = C.sb("num", [64, 256], stack=st2)
        den = C.sb("den", [64, 4], stack=st2); hraw = C.sb("hraw", [64, 256], stack=st2)
        sqj = C.sb("sqj", [64, 256], stack=st2); sig = C.sb("sig", [64, 256], stack=st2)
        hmb = C.sb("hmb", [64, 256], BF16, stack=st2)
        epsm = C.sb("epsm", [64, 1], stack=st2)
        C.memset(epsm[:], EPS, ["epsm"])

        def chunk(hp, col0, L, full, mixcol):
            hk = hkeys(col0, L)
            pm = ps[2]
            for kt in range(KT):
                C.mm(pm[0:L, 0:8], hT[:, kt, col0:col0 + L], wgate[:, kt, :], kt == 0, kt == KT - 1, hk + ["wgate"],
                     [("ps", 2)])
            C.tt(gts[0:L, :], pm[0:L, 0:8], rc[0:L, 0:8], ALU.add, [("ps", 2), "rc"], ["gts"])
            C.act(lf[0:L, :], gts[0:L, 4:8], ACT.Exp, ["gts"], ["lf"], scale=-1.0)
            C.act(lf[0:L, :], lf[0:L, :], ACT.Ln, ["lf"], ["lf"], bias=ONE_AP[0:L, 0:1], scale=1.0)
            C.ts(lf[0:L, :], lf[0:L, :], -1.0, ALU.mult, ["lf"], ["lf"])
            C.mm(pm[0:L, 16:20], Utri[0:L, 0:L], lf[0:L, :], True, True, ["Utri", "lf"], [("ps", 2)])
            C.mm(pm[:, 24:28], onesf[0:L, :], lf[0:L, :], True, True, ["onesf", "lf"], [("ps", 2)])
            C.cp(bsb[0:L, :], pm[0:L, 16:20], [("ps", 2)], ["bsb"])
            C.cp(bl[:, :], pm[:, 24:28], [("ps", 2)], ["bl"])
            C.tt(u[0:L, :], gts[0:L, 0:4], bsb[0:L, :], ALU.subtract, ["gts", "bsb"], ["u"])
            for hl in range(2):
                h = hp * 2 + hl
                C.ts(Dg[0:L, hl, 0:L], ident[0:L, 0:L], u[0:L, h:h + 1], ALU.mult, ["ident", "u"], ["Dg"])
            pM = ps[3]
            for hl in range(2):
                C.mm(pM[:, hl * 64:hl * 64 + L], onesf[0:L, :], Dg[0:L, hl, 0:L], True, True, ["onesf", "Dg"], [("ps", 3)])
            for hl in range(2):
                C.red(mmax[:, hl:hl + 1], pM[:, hl * 64:hl * 64 + L], ALU.max, [("ps", 3)], ["mmax"])
            C.tt(t2[:, :], mrep[:, :], mmax[:, :], ALU.max, ["mrep", "mmax"], ["t2"])
            C.tt(mnew[:, :], t2[:, :], bl[:, hp * 2:hp * 2 + 2], ALU.add, ["t2", "bl"], ["mnew"])
            C.tt(wsv[0:L, :], u[0:L, hp * 2:hp * 2 + 2], bl[0:L, hp * 2:hp * 2 + 2], ALU.add, ["u", "bl"], ["wsv"])
            C.tt(wsv[0:L, :], wsv[0:L, :], mnew[0:L, :], ALU.subtract, ["wsv", "mnew"], ["wsv"])
            C.act(wsv[0:L, :], wsv[0:L, :], ACT.Exp, ["wsv"], ["wsv"])
            C.cp(wsb[0:L, :], wsv[0:L, :], ["wsv"], ["wsb"])
            C.tt(alast[:, :], bl[:, hp * 2:hp * 2 + 2], mrep[:, :], ALU.add, ["bl", "mrep"], ["alast"])
            C.tt(alast[:, :], alast[:, :], mnew[:, :], ALU.subtract, ["alast", "mnew"], ["alast"])
            C.act(alast[:, :], alast[:, :], ACT.Exp, ["alast"], ["alast"])
            pk_ = ps[0]; pv_ = ps[1]
            for kt in range(KT):
                C.mm(pk_[0:L, :], hT[:, kt, col0:col0 + L], WK[0][:, kt, :], kt == 0, kt == KT - 1, hk + WK[1], [("ps", 0)])
            for kt in range(KT):
                C.mm(pv_[0:L, :], hT[:, kt, col0:col0 + L], WV[0][:, kt, :], kt == 0, kt == KT - 1, hk + WV[1], [("ps", 1)])
            C.act(ktok[0:L, :], pk_[0:L, :], ACT.Identity, [("ps", 0)], ["ktok"], scale=1.0 / 16.0)
            for hl in range(2):
                C.ts(vws[0:L, hl, :], pv_[0:L, hl * 256:(hl + 1) * 256], wsv[0:L, hl:hl + 1], ALU.mult, [("ps", 1), "wsv"],
                     ["vws"])
            if full:
                C.cp(vbf[0:L, :, :], pv_[0:L, :].rearrange("p (a b) -> p a b", a=2), [("ps", 1)], ["vbf"], eng="scalar")
                po_ = ps[4]
                for kt in range(KT):
                    C.mm(po_[0:L, :], hT[:, kt, col0:col0 + L], WO[0][:, kt, :], kt == 0, kt == KT - 1, hk + WO[1],
                         [("ps", 4)])
                qc0 = mixcol
                kc0 = col0
                for hl in range(2):
                    h = hp * 2 + hl
                    C.stt(dm[0:L, hl, 0:L], pM[0:L, hl * 64:hl * 64 + L], bsb[0:L, h:h + 1], negm[0:L, 0:L], ALU.add, ALU.add,
                          [("ps", 3), "bsb", "negm"], ["dm"])
                    C.red(rowmax[0:L, hl:hl + 1], dm[0:L, hl, 0:L], ALU.max, ["dm"], ["rowmax"])
                C.tt(t3[0:L, :], bsb[0:L, hp * 2:hp * 2 + 2], mrep[0:L, :], ALU.add, ["bsb", "mrep"], ["t3"])
                C.tt(mt[0:L, :], t3[0:L, :], rowmax[0:L, :], ALU.max, ["t3", "rowmax"], ["mt"])
                C.tt(t3[0:L, :], t3[0:L, :], mt[0:L, :], ALU.subtract, ["t3", "mt"], ["t3"])
                C.act(av[0:L, :], t3[0:L, :], ACT.Exp, ["t3"], ["av"])
                C.ts(mt[0:L, :], mt[0:L, :], -1.0, ALU.mult, ["mt"], ["mt"])
                C.act(em[0:L, :], mt[0:L, :], ACT.Exp, ["mt"], ["em"])
                for hl in range(2):
                    C.act(wmat[0:L, hl, 0:L], dm[0:L, hl, 0:L], ACT.Exp, ["dm", "mt"], ["wmat"], bias=mt[0:L, hl:hl + 1],
                          scale=1.0)
                pq = ps[5]
                for hl in range(2):
                    for dt in range(2):
                        C.mm(pq[0:L, hl * 64:hl * 64 + L], qmT[:, hl * 2 + dt, qc0:qc0 + L], kmT[:, hl * 2 + dt, kc0:kc0 + L],
                             dt == 0, dt == 1, ["qmT", "kmTm"], [("ps", 5)])
                for hl in range(2):
                    C.tt(sc[0:L, hl, 0:L], pq[0:L, hl * 64:hl * 64 + L], wmat[0:L, hl, 0:L], ALU.mult, [("ps", 5), "wmat"], ["sc"])
                    C.red(dsum[0:L, hl:hl + 1], sc[0:L, hl, 0:L], ALU.add, ["sc"], ["dsum"])
                pT = ps[6]
                for hl in range(2):
                    C.tr(pT[0:L, hl * 64:hl * 64 + L], sc[0:L, hl, 0:L], ident[0:L, 0:L], ["sc", "ident"], [("ps", 6)])
                for hl in range(2):
                    C.cp(scT[0:L, hl, 0:L], pT[0:L, hl * 64:hl * 64 + L], [("ps", 6)], ["scT"], eng="scalar")
                for hl in range(2):
                    h = hp * 2 + hl
                    pin = ps[5]
                    for dt in range(2):
                        C.mm(pin[0:L, 256:512], qmT[:, hl * 2 + dt, qc0:qc0 + L], Sb[:, hl * 2 + dt, :], dt == 0, dt == 1,
                             ["qmT", "Sb"], [("ps", 5, "b")])
                    for dt in range(2):
                        C.mm(pm[0:L, 32 + hl:33 + hl], qmT[:, hl * 2 + dt, qc0:qc0 + L], nSb[:, hl * 2 + dt:hl * 2 + dt + 1],
                             dt == 0, dt == 1, ["qmT", "nSb"], [("ps", 2, "nq")])
                    pia = ps[6]
                    C.mm(pia[0:L, 256:512], scT[0:L, hl, 0:L], vbf[0:L, hl, :], True, True, ["scT", "vbf"], [("ps", 6, "b")])
                    C.act(tnum[0:L, :], pin[0:L, 256:512], ACT.Identity, [("ps", 5, "b"), "av"], ["tnum"], scale=av[0:L, hl:hl + 1])
                    C.tt(num[0:L, :], tnum[0:L, :], pia[0:L, 256:512], ALU.add, ["tnum", ("ps", 6, "b")], ["num"])
                    C.stt(den[0:L, 0:1], pm[0:L, 32 + hl:33 + hl], av[0:L, hl:hl + 1], dsum[0:L, hl:hl + 1], ALU.mult, ALU.add,
                          [("ps", 2, "nq"), "av", "dsum"], ["den"])
                    C.ts(den[0:L, 1:2], den[0:L, 0:1], -1.0, ALU.mult, ["den"], ["den1"])
                    C.tt(den[0:L, 0:1], den[0:L, 0:1], den[0:L, 1:2], ALU.max, ["den", "den1"], ["den"])
                    C.tt(den[0:L, 0:1], den[0:L, 0:1], em[0:L, hl:hl + 1], ALU.max, ["den", "em"], ["den"])
                    C.recip(den[0:L, 1:2], den[0:L, 0:1], ["den"], ["den1"])
                    C.ts(hraw[0:L, :], num[0:L, :], den[0:L, 1:2], ALU.mult, ["num", "den1"], ["hraw"])
                    C.act(sqj[0:L, :], hraw[0:L, :], ACT.Square, ["hraw"], ["sqj"])
                    C.red(den[0:L, 2:3], sqj[0:L, :], ALU.add, ["sqj"], ["den2"])
                    C.act(den[0:L, 2:3], den[0:L, 2:3], ACT.Sqrt, ["den2"], ["den2"], bias=epsm[0:L, 0:1], scale=1.0 / 256.0)
                    C.recip(den[0:L, 3:4], den[0:L, 2:3], ["den2"], ["den3"])
                    C.act(sig[0:L, :], po_[0:L, hl * 256:(hl + 1) * 256], ACT.Sigmoid, [("ps", 4)], ["sig"])
                    C.tt(sig[0:L, :], sig[0:L, :], rc[0:L, 28 + h * 256:28 + (h + 1) * 256], ALU.mult, ["sig", "rc"], ["sig"])
                    C.stt(hmb[0:L, :], hraw[0:L, :], den[0:L, 3:4], sig[0:L, :], ALU.mult, ALU.mult, ["hraw", "den3", "sig"],
                          ["hmb"])
                    for dt in range(2):
                        C.tr(psb[:, dt * 64:dt * 64 + L], hmb[0:L, dt * 128:(dt + 1) * 128], identb[0:L, 0:L],
                             ["hmb", "identb"], [("psb",)])
                    for dt in range(2):
                        C.cp(mixT[:, 8 + 2 * h + dt, mixcol:mixcol + L], psb[:, dt * 64:dt * 64 + L], [("psb",)],
                             [("mix", 8 + 2 * h + dt)], eng="scalar")
            pS = [ps[5], ps[6]] if not full else [ps[0], ps[1]]
            pSk = [("ps", 5), ("ps", 6)] if not full else [("ps", 0), ("ps", 1)]
            pSw = [[("ps", 5), ("ps", 5, "b")], [("ps", 6), ("ps", 6, "b")]] if not full else [[("ps", 0)], [("ps", 1)]]
            for hl in range(2):
                for dt in range(2):
                    C.mm(pS[hl][:, dt * 256:(dt + 1) * 256], ktok[0:L, hl * 256 + dt * 128:hl * 256 + (dt + 1) * 128],
                         vws[0:L, hl, :], True, True, ["ktok", "vws"], pSw[hl])
                    C.mm(pm[:, 40 + hl * 2 + dt:41 + hl * 2 + dt], ktok[0:L, hl * 256 + dt * 128:hl * 256 + (dt + 1) * 128],
                         wsb[0:L, hl:hl + 1], True, True, ["ktok", "wsb"], [("ps", 2, "dn")])
            for hl in range(2):
                for dt in range(2):
                    i = hl * 2 + dt
                    C.stt(S[:, i, :], S[:, i, :], alast[:, hl:hl + 1], pS[hl][:, dt * 256:(dt + 1) * 256], ALU.mult, ALU.add,
                          ["S", "alast", pSk[hl]], ["S"])
                    C.stt(nS[:, i:i + 1], nS[:, i:i + 1], alast[:, hl:hl + 1], pm[:, 40 + i:41 + i], ALU.mult, ALU.add,
                          ["nS", "alast", ("ps", 2, "dn")], ["nS"])
            C.cp(Sb[:, :, :], S[:, :, :], ["S"], ["Sb"], eng="scalar")
            C.cp(nSb[:, :], nS[:, :], ["nS"], ["nSb"])
            C.cp(mrep[:, :], mnew[:, :], ["mnew"], ["mrep"])

        ONE_AP = C.sb("oneap", [64, 1], stack=st2)
        C.memset(ONE_AP[:], 1.0, ["oneap"])
        WK = [None, None]; WV = [None, None]; WO = [None, None]
        for hp in range(2):
            Wq, wqk = C.load_w(winv[:, :, 3072 + hp * 512:3072 + (hp + 1) * 512])
            for i in range(4):
                for (c0, n, m0) in [(1024, 512, 0), (1536, 512, 512), (2048, 32, 1024)]:
                    pb = ps[(i + c0 // 512) % 2]; pk = ("ps", (i + c0 // 512) % 2)
                    for kt in range(KT):
                        C.mm(pb[:, 0:n], Wq[:, kt, i * 128:(i + 1) * 128], hT[:, kt, c0:c0 + n], kt == 0, kt == KT - 1,
                             wqk + hkeys(c0, n), [pk])
                    C.cp(qmT[:, i, m0:m0 + n], pb[:, 0:n], [pk], ["qmT"], eng="scalar")
            Wk_, wkk = C.load_w(winv[:, :, 4096 + hp * 512:4096 + (hp + 1) * 512])
            for i in range(4):
                for (c0, n) in [(0, 512), (512, 512), (1024, 512), (1536, 512), (2048, 32)]:
                    pb = ps[(i + c0 // 512) % 2]; pk = ("ps", (i + c0 // 512) % 2)
                    for kt in range(KT):
                        C.mm(pb[:, 0:n], Wk_[:, kt, i * 128:(i + 1) * 128], hT[:, kt, c0:c0 + n], kt == 0, kt == KT - 1,
                             wkk + hkeys(c0, n), [pk])
                    C.act(kmT[:, i, c0:c0 + n], pb[:, 0:n], ACT.Identity, [pk], ["kmTm"], scale=1.0 / 16.0)
            WK[0], WK[1] = Wk_, wkk
            WV[0], WV[1] = C.load_w(winv[:, :, 5120 + hp * 512:5120 + (hp + 1) * 512])
            C.memset(S[:, :, :], 0.0, ["S"]); C.memset(nS[:, :], 0.0, ["nS"]); C.memset(mrep[:, :], 0.0, ["mrep"])
            C.cp(Sb[:, :, :], S[:, :, :], ["S"], ["Sb"], eng="scalar"); C.cp(nSb[:, :], nS[:, :], ["nS"], ["nSb"])
            for c in range(16):
                chunk(hp, c * 64, 64, False, None)
            C.ts(S[:, :, :], S[:, :, :], flg[:, 0:1], ALU.mult, ["S", "flg"], ["S"])
            C.ts(nS[:, :], nS[:, :], flg[:, 0:1], ALU.mult, ["nS", "flg"], ["nS"])
            C.ts(mrep[:, :], mrep[:, :], flg[:, 0:1], ALU.mult, ["mrep", "flg"], ["mrep"])
            C.cp(Sb[:, :, :], S[:, :, :], ["S"], ["Sb"], eng="scalar"); C.cp(nSb[:, :], nS[:, :], ["nS"], ["nSb"])
            WO[0], WO[1] = C.load_w(winv[:, :, 6144 + hp * 512:6144 + (hp + 1) * 512])
            for c in range(16, 32):
                chunk(hp, c * 64, 64, True, (c - 16) * 64)
            for hl in range(2):
                h = hp * 2 + hl
                C.st(Cp[h].rearrange("(dt p) v -> p dt v", p=128), S[:, hl * 2:hl * 2 + 2, :], ["S"], [("Cp", h)], key=("Sst", "C", hl))
                C.st(npo[h].rearrange("(dt p) -> p dt", p=128), nS[:, hl * 2:hl * 2 + 2], ["nS"], [("np", h)], key=("Sst", "n", hl),
                     slow=True)
            C.st(mpo[0:1, hp * 2:hp * 2 + 2], mrep[0:1, :], ["mrep"], [("mp", hp)], key=("Sst", "m"))
            for j in range(4):
                for hl in range(2):
                    h = hp * 2 + hl
                    C.ld(S[:, hl * 2:hl * 2 + 2, :], sC[j, h].rearrange("(dt p) v -> p dt v", p=128), ["S"], key=("Sld", hl))
                    C.ld(nS[:, hl * 2:hl * 2 + 2], sn[j, h].rearrange("(dt p) -> p dt", p=128), ["nS"], key=("Sld", 2 + hl),
                         slow=True)
                C.cp(mrep[:, :], smt[:, j * 4 + hp * 2:j * 4 + hp * 2 + 2], ["smt"], ["mrep"])
                C.cp(Sb[:, :, :], S[:, :, :], ["S"], ["Sb"], eng="scalar"); C.cp(nSb[:, :], nS[:, :], ["nS"], ["nSb"])
                chunk(hp, 2048 + 8 * j, 8, True, 1024 + 8 * j)
                for hl in range(2):
                    h = hp * 2 + hl
                    C.st(Cs[j, h].rearrange("(dt p) v -> p dt v", p=128), S[:, hl * 2:hl * 2 + 2, :], ["S"], [("Cs", j, h)],
                         key=("Sst", "C", hl))
                    C.st(nso[j, h].rearrange("(dt p) -> p dt", p=128), nS[:, hl * 2:hl * 2 + 2], ["nS"], [("ns", j, h)],
                         key=("Sst", "n", hl), slow=True)
                C.st(mso[0:1, j * 4 + hp * 2:j * 4 + hp * 2 + 2], mrep[0:1, :], ["mrep"], [("ms", j, hp)], key=("Sst", "m"))


def _phase_tail(C, ps, R1, mixT, xo, xs, atts, wout, wg, wu, wd, wrb, rc, gv, modT, A2, onesb, onesf, ident, c_sel, EPS_AP,
                yo, ys, dbg):
    P = C.P
    x1 = R1[:].bitcast(F32)[:, 0:KT * NMY].rearrange("p (k n) -> p k n", k=KT)
    h2T = mixT
    HKall = [("Bdst", gi) for gi in range(5)]
    MIXK = [("mix", f) for f in range(16)] + [("mix", "smp")]
    TG = [(0, 512), (512, 512), (1024, 32)]
    with contextlib.ExitStack() as st2:
        selT = C.sb("selT", [16, 16 * 128], stack=st2)
        C.ld(selT[:], c_sel, ["selT"])
        rstd = C.sb("rstd", [128, NMY], stack=st2)
        sq = [C.sb(f"T_sq{i}", [128, 512], BF16, stack=st2) for i in range(2)]
        tm = [C.sb(f"T_tm{i}", [128, 512], stack=st2) for i in range(2)]
        C.ld(mixT[:, 0:8, 1024:1056], atts.rearrange("(f p) n -> p f n", p=128), [("mix", "smp")], eng="gpsimd")
        woutv = wout.rearrange("(k p) c -> p k c", p=128)
        cnt = 0
        xk = []
        for dt in range(KT):
            first = [("Bdst", gi) for gi in range(5)] if dt == 0 else []
            C.ld(x1[:, dt, 0:1024], xo[dt * 128:(dt + 1) * 128, :], [("x1", dt)] + first, key=("x1ld",))
            C.ld(x1[:, dt, 1024:1056], xs[dt * 128:(dt + 1) * 128, :], [("x1", dt, "s")], key=("x1ld",))
            xk += [("x1", dt), ("x1", dt, "s")]
        P.unify(("x1ld",), xk)
        for g in range(4):
            W, wk = C.load_w(woutv[:, :, g * 512:(g + 1) * 512])
            for j in range(4):
                dt = g * 4 + j
                for (c0, n) in TG:
                    pb = ps[cnt % 2]; pk = ("ps", cnt % 2); cnt += 1
                    for ft in range(KT):
                        C.mm(pb[:, 0:n], W[:, ft, j * 128:(j + 1) * 128], mixT[:, ft, c0:c0 + n], ft == 0, ft == KT - 1,
                             wk + MIXK, [pk])
                    if n == 512:
                        C.stt(x1[:, dt, c0:c0 + n], pb[:, 0:n], modT[:, 32 + dt, 0:1], x1[:, dt, c0:c0 + n], ALU.mult, ALU.add,
                              [pk, "modT", ("x1", dt)], [("x1", dt)])
                    else:
                        for jb in range(4):
                            C.stt(x1[:, dt, c0 + 8 * jb:c0 + 8 * jb + 8], pb[:, 8 * jb:8 * jb + 8], modT[:, 32 + dt, 1 + jb:2 + jb],
                                  x1[:, dt, c0 + 8 * jb:c0 + 8 * jb + 8], ALU.mult, ALU.add, [pk, "modT", ("x1", dt, "s")],
                                  [("x1", dt, "s"), ("x1", dt)])
        X1K = [("x1", dt) for dt in range(KT)]

        def rms_stats():
            for gi, (c0, n) in enumerate(TG):
                pacc = ps[2 + gi % 2]; pk = ("ps", 2 + gi % 2)
                for ft in range(KT):
                    C.act(sq[ft % 2][:, 0:n], x1[:, ft, c0:c0 + n], ACT.Square, [("x1", ft)], [("Tsq", ft % 2)])
                    C.mm(pacc[:, 0:n], onesb[:], sq[ft % 2][:, 0:n], ft == 0, ft == KT - 1, [("Tsq", ft % 2), "onesb"], [pk])
                C.act(rstd[:, c0:c0 + n], pacc[:, 0:n], ACT.Sqrt, [pk, "eps"], [("rstd", gi)], bias=EPS_AP[:, 0:1], scale=1.0 / D)
                C.recip(rstd[:, c0:c0 + n], rstd[:, c0:c0 + n], [("rstd", gi)], [("rstd", gi)])

        rms_stats()
        for ft in range(KT):
            for gi, (c0, n) in enumerate(TG):
                t = tm[(ft * 3 + gi) % 2]; tk = ("Ttm", (ft * 3 + gi) % 2)
                C.tt(t[:, 0:n], x1[:, ft, c0:c0 + n], rstd[:, c0:c0 + n], ALU.mult, [("x1", ft), ("rstd", gi)], [tk])
                rows = [(0, 512, 0)] if n == 512 else [(8 * jb, 8, 1 + jb) for jb in range(4)]
                for (r0, rn, r) in rows:
                    C.act(h2T[:, ft, c0 + r0:c0 + r0 + rn], t[:, r0:r0 + rn], ACT.Identity, [tk, "A2", "modT"],
                          [("h2", gi)] + (MIXK if ft == 0 else []), bias=modT[:, 48 + ft, r:r + 1], scale=A2[:, ft, r:r + 1])
        H2K = [("h2", gi) for gi in range(3)]

        lg = C.sb("lg", [128, 20], stack=st2); r1 = C.sb("r1", [128, 8], stack=st2)
        eg = C.sb("eg", [128, 4], stack=st2); oh = C.sb("oh", [128, 4], stack=st2)
        el8 = C.sb("el8", [128, 8], stack=st2); t8 = C.sb("t8", [128, 8], stack=st2)
        sele = C.sb("sele", [128, 4], stack=st2); ee = C.sb("ee", [128, 4], stack=st2)
        g16 = C.sb("g16", [128, 16], stack=st2)
        gT = C.sb("gT", [16, NMY], stack=st2)
        C.memset(el8[:], -BIG, ["el8"])
        TT = [(i * 128, 128) for i in range(8)] + [(1024, 32)]
        for ti, (t0, n) in enumerate(TT):
            pl = ps[4]
            for kt in range(KT):
                C.mm(pl[0:n, 0:20], h2T[:, kt, t0:t0 + n], wrb[:, kt, :], kt == 0, kt == KT - 1, H2K + ["wrb"], [("ps", 4)])
            C.tt(lg[0:n, :], pl[0:n, 0:20], rc[0:n, 8:28], ALU.add, [("ps", 4), "rc"], ["lg"])
            C.red(r1[0:n, 0:1], lg[0:n, 0:4], ALU.max, ["lg"], ["r1a"])
            C.ts(r1[0:n, 1:2], r1[0:n, 0:1], -1.0, ALU.mult, ["r1a"], ["r1b"])
            C.act(eg[0:n, :], lg[0:n, 0:4], ACT.Exp, ["lg", "r1b"], ["eg"], bias=r1[0:n, 1:2], scale=1.0)
            C.red(r1[0:n, 2:3], eg[0:n, :], ALU.add, ["eg"], ["r1c"])
            C.recip(r1[0:n, 3:4], r1[0:n, 2:3], ["r1c"], ["r1d"])
            C.ts(oh[0:n, :], lg[0:n, 0:4], r1[0:n, 0:1], ALU.is_ge, ["lg", "r1a"], ["oh"])
            C.ts(el8[0:n, 0:4], lg[0:n, 4:8], oh[0:n, 0:1], ALU.mult, ["lg", "oh"], ["el8"])
            for g in range(1, 4):
                C.stt(el8[0:n, 0:4], lg[0:n, 4 + 4 * g:8 + 4 * g], oh[0:n, g:g + 1], el8[0:n, 0:4], ALU.mult, ALU.add,
                      ["lg", "oh", "el8"], ["el8"])
            P.op("vector", lambda e, o=t8[0:n, :], i=el8[0:n, :]: e.max(o, i), ["el8"], ["t8"])
            C.ts(sele[0:n, :], el8[0:n, 0:4], t8[0:n, 1:2], ALU.is_ge, ["el8", "t8"], ["sele"])
            C.ts(r1[0:n, 4:5], t8[0:n, 0:1], -1.0, ALU.mult, ["t8"], ["r1e"])
            C.act(ee[0:n, :], el8[0:n, 0:4], ACT.Exp, ["el8", "r1e"], ["ee"], bias=r1[0:n, 4:5], scale=1.0)
            C.tt(ee[0:n, :], ee[0:n, :], sele[0:n, :], ALU.mult, ["ee", "sele"], ["ee"])
            C.red(r1[0:n, 5:6], ee[0:n, :], ALU.add, ["ee"], ["r1f"])
            C.recip(r1[0:n, 6:7], r1[0:n, 5:6], ["r1f"], ["r1g"])
            C.ts(ee[0:n, :], ee[0:n, :], r1[0:n, 6:7], ALU.mult, ["ee", "r1g"], ["ee"])
            C.ts(oh[0:n, :], oh[0:n, :], r1[0:n, 3:4], ALU.mult, ["oh", "r1d"], ["oh"])
            for g in range(4):
                C.ts(g16[0:n, 4 * g:4 * g + 4], ee[0:n, :], oh[0:n, g:g + 1], ALU.mult, ["ee", "oh"], ["g16"])
            C.tr(pl[0:16, 256:256 + n], g16[0:n, :], ident[0:n, 0:n], ["g16", "ident"], [("ps", 4, "t")])
            C.cp(gT[0:16, t0:t0 + n], pl[0:16, 256:256 + n], [("ps", 4, "t")], ["gT"])
        if "gT" in dbg:
            o = C.outp("d_gT", [16, NMY]); C.st(o, gT[:, :], ["gT"], ["d_gT"])

        actT = [C.sb(f"actT{i}", [128, 4, NMY], BF16, stack=st2) for i in range(1)]
        sl = [C.sb(f"sl{i}", [128, 512], stack=st2) for i in range(2)]
        grep_ = C.sb("grep", [128, NMY], stack=st2)
        it = 0
        dcnt = 0
        for e in range(16):
            Wg, wgk = C.load_w(wg[e].rearrange("(k p) c -> p k c", p=128))
            Wu, wuk = C.load_w(wu[e].rearrange("(k p) c -> p k c", p=128))
            Wd, wdk = C.load_w(wd[e].rearrange("(j p) d -> p j d", p=128))
            aT = actT[0]
            for gi, (c0, n) in enumerate(TG):
                pgr = ps[4]
                C.mm(pgr[:, 0:n], selT[0:16, e * 128:(e + 1) * 128], gT[0:16, c0:c0 + n], True, True, ["selT", "gT"],
                     [("ps", 4), ("ps", 4, "t")])
                C.cp(grep_[:, c0:c0 + n], pgr[:, 0:n], [("ps", 4)], [("grep", gi)], eng="scalar")
            for j in range(4):
                for gi, (c0, n) in enumerate(TG):
                    b0 = (it % 2) * 2; it += 1
                    pa = ps[b0]; pu = ps[b0 + 1]
                    for kt in range(KT):
                        C.mm(pa[:, 0:n], Wg[:, kt, j * 128:(j + 1) * 128], h2T[:, kt, c0:c0 + n], kt == 0, kt == KT - 1,
                             wgk + H2K, [("ps", b0)])
                    for kt in range(KT):
                        C.mm(pu[:, 0:n], Wu[:, kt, j * 128:(j + 1) * 128], h2T[:, kt, c0:c0 + n], kt == 0, kt == KT - 1,
                             wuk + H2K, [("ps", b0 + 1)])
                    s_ = sl[it % 2]; sk = ("sl", it % 2)
                    C.act(s_[:, 0:n], pa[:, 0:n], ACT.Silu, [("ps", b0)], [sk])
                    C.tt(s_[:, 0:n], s_[:, 0:n], pu[:, 0:n], ALU.mult, [sk, ("ps", b0 + 1)], [sk])
                    C.tt(aT[:, j, c0:c0 + n], s_[:, 0:n], grep_[:, c0:c0 + n], ALU.mult, [sk, ("grep", gi)], [("actT", j)],
                         )
            for dt in range(KT):
                for gi, (c0, n) in enumerate(TG):
                    bk = 5 + dcnt % 2; dcnt += 1
                    pd_ = ps[bk]
                    for j in range(4):
                        C.mm(pd_[:, 0:n], Wd[:, j, dt * 128:(dt + 1) * 128], aT[:, j, c0:c0 + n], j == 0, j == 3,
                             wdk + [("actT", j)], [("ps", bk)])
                    if n == 512:
                        C.stt(x1[:, dt, c0:c0 + n], pd_[:, 0:n], modT[:, 80 + dt, 0:1], x1[:, dt, c0:c0 + n], ALU.mult, ALU.add,
                              [("ps", bk), "modT", ("x1", dt)], [("x1", dt)])
                    else:
                        for jb in range(4):
                            C.stt(x1[:, dt, c0 + 8 * jb:c0 + 8 * jb + 8], pd_[:, 8 * jb:8 * jb + 8], modT[:, 80 + dt, 1 + jb:2 + jb],
                                  x1[:, dt, c0 + 8 * jb:c0 + 8 * jb + 8], ALU.mult, ALU.add, [("ps", bk), "modT", ("x1", dt)],
                                  [("x1", dt)])

        rms_stats()
        for ft in range(KT):
            C.stt(x1[:, ft, 0:512], x1[:, ft, 0:512], gv[:, 32 + ft:33 + ft], rstd[:, 0:512], ALU.mult, ALU.mult,
                  [("x1", ft), "gv", ("rstd", 0)], [("x1", ft)])
            C.stt(x1[:, ft, 512:NMY], x1[:, ft, 512:NMY], gv[:, 32 + ft:33 + ft], rstd[:, 512:NMY], ALU.mult, ALU.mult,
                  [("x1", ft), "gv", ("rstd", 1), ("rstd", 2)], [("x1", ft)])
            C.st(yo[ft * 128:(ft + 1) * 128, :], x1[:, ft, 0:1024], [("x1", ft)], [("yo", ft)], key=("yst",))
            C.st(ys[ft * 128:(ft + 1) * 128, :], x1[:, ft, 1024:1056], [("x1", ft)], [("ys", ft)], key=("yst",))


NTS = 256
NPG = 64
NBLK = 32


def build_samp(nb=32, dbg=()):
    nc = bass.Bass("TRN2", target_bir_lowering=False)
    stack = contextlib.ExitStack()
    with stack:
        C = Ctx(nc, stack)
        P = C.P
        xs = C.inp("xs", [D, NTS]); cT = C.inp("cT", [D, 32])
        wmod = C.inp("wmod", [D, 4096]); bmodT = C.inp("bmodT", [128, 32]); gmix = C.inp("gmix", [128, 16])
        wqkv = C.inp("wqkv", [D, 384])
        ck = C.inp("ck", [2560 * 128, 128]); cv = C.inp("cv", [2560 * 128, 128])
        ptabT = C.inp("ptabT", [NPG, 32], I32)
        c_ident = C.inp("c_ident", [128, 128]); c_pair = C.inp("c_pair", [64, NBLK]); c_indj = C.inp("c_indj", [128, 64])
        c_alib = C.inp("c_alib", [128, 1024]); c_nb = C.inp("c_nb", [128, 8])
        attT = C.outp("attT", [128, NTS]); kso = C.outp("kso", [NTS, 128]); vso = C.outp("vso", [NTS, 128])

        C.alloc_wbufs(2)
        ident = C.sb("ident", [128, 128]); identb = C.sb("identb", [128, 128], BF16)
        onesb = C.sb("onesb", [128, 128], BF16); onesf = C.sb("onesf", [128, 128])
        alib = C.sb("alib", [128, 1024]); nbias = C.sb("nbias", [128, 8])
        gv = C.sb("gv", [128, 16]); bmod = C.sb("bmod", [128, 32])
        cTb = C.sb("cTb", [128, KT, 32], BF16); wq = C.sb("wq", [128, KT, 384], BF16)
        modT = C.sb("modT", [128, 32, 32]); A1 = C.sb("A1", [128, 16, 32])
        hT = C.sb("hT", [128, KT, NTS], BF16)
        epsap = C.sb("epsap", [128, 1])
        ps = [C.psum(f"ps{i}", [128, 512]) for i in range(7)]
        psb = C.psum("psb", [128, 1024], BF16)

        ckk = []
        for (dst, src, k) in [(ident[:], c_ident, "ident"), (alib[:], c_alib, "alib"), (nbias[:], c_nb, "nbias"),
                              (gv[:], gmix, "gv"), (bmod[:], bmodT, "bmod")]:
            C.ld(dst, src, [k], key=("constS",)); ckk.append(k)
        P.unify(("constS",), ckk)
        ckk = []
        for (dst, src, k) in [(identb[:], c_ident, "identb"),
                              (cTb[:], cT.rearrange("(k p) r -> p k r", p=128), "cTb"),
                              (wq[:], wqkv.rearrange("(k p) c -> p k c", p=128), "wq")]:
            C.ld(dst, src, [k], eng="gpsimd", key=("constG",)); ckk.append(k)
        P.unify(("constG",), ckk)
        C.memset(onesb[:], 1.0, ["onesb"]); C.memset(onesf[:], 1.0, ["onesf"]); C.memset(epsap[:], EPS, ["eps"])

        wmv = wmod.rearrange("(k p) c -> p k c", p=128)
        for g in range(8):
            W, wk = C.load_w(wmv[:, :, g * 512:(g + 1) * 512])
            pb = ps[g % 2]
            for j in range(4):
                for kt in range(KT):
                    C.mm(pb[:, j * 32:(j + 1) * 32], W[:, kt, j * 128:(j + 1) * 128], cTb[:, kt, :], kt == 0, kt == KT - 1,
                         wk + ["cTb"], [("ps", g % 2)])
            for j in range(4):
                ft = g * 4 + j
                C.ts(modT[:, ft, :], pb[:, j * 32:(j + 1) * 32], bmod[:, ft:ft + 1], ALU.add, [("ps", g % 2), "bmod"], ["modT"])
        for ft in range(KT):
            C.ts(A1[:, ft, :], modT[:, 16 + ft, :], 1.0, ALU.add, ["modT", "gv"], ["A1"], s2=gv[:, ft:ft + 1], op1=ALU.mult)

        with contextlib.ExitStack() as st2:
            xt = [C.sb(f"xt{i}", [128, NTS], stack=st2) for i in range(3)]
            sq = [C.sb(f"sq{i}", [128, NTS], BF16, stack=st2) for i in range(2)]
            rsd = C.sb("rsd", [128, NTS], stack=st2)
            tm = [C.sb(f"tm{i}", [128, NTS], stack=st2) for i in range(2)]
            cnt = 0
            for ft in range(KT):
                b = cnt % 3; cnt += 1
                C.ld(xt[b][:], xs[ft * 128:(ft + 1) * 128, :], [("xt", b)])
                C.act(sq[ft % 2][:], xt[b][:], ACT.Square, [("xt", b)], [("sq", ft % 2)])
                C.mm(ps[2][:, 0:NTS], onesb[:], sq[ft % 2][:], ft == 0, ft == KT - 1, [("sq", ft % 2), "onesb"], [("ps", 2)])
            C.act(rsd[:], ps[2][:, 0:NTS], ACT.Sqrt, [("ps", 2), "eps"], ["rsd"], bias=epsap[:, 0:1], scale=1.0 / D)
            C.recip(rsd[:], rsd[:], ["rsd"], ["rsd"])
            for ft in range(KT):
                b = cnt % 3; cnt += 1
                C.ld(xt[b][:], xs[ft * 128:(ft + 1) * 128, :], [("xt", b)])
                t = tm[ft % 2]
                C.tt(t[:], xt[b][:], rsd[:], ALU.mult, [("xt", b), "rsd"], [("tm", ft % 2)])
                for bb in range(32):
                    C.act(hT[:, ft, bb * 8:(bb + 1) * 8], t[:, bb * 8:(bb + 1) * 8], ACT.Identity, [("tm", ft % 2), "A1", "modT"],
                          ["hT"], bias=modT[:, ft, bb:bb + 1], scale=A1[:, ft, bb:bb + 1])
        P.barrier()

        qf = C.sb("qf", [128, NTS]); qb = C.sb("qb", [128, NTS], BF16); kTb = C.sb("kTb", [128, NTS], BF16)
        stg = [C.sb(f"stg{i}", [128, 128]) for i in range(2)]
        for kt in range(KT):
            C.mm(ps[0][:, 0:NTS], wq[:, kt, 0:128], hT[:, kt, :], kt == 0, kt == KT - 1, ["wq", "hT"], [("ps", 0)])
        C.cp(qf[:], ps[0][:, 0:NTS], [("ps", 0)], ["qf"])
        C.act(qb[:], ps[0][:, 0:NTS], ACT.Identity, [("ps", 0)], ["qb"], scale=SCALE_A)
        for kt in range(KT):
            C.mm(ps[1][:, 0:NTS], wq[:, kt, 128:256], hT[:, kt, :], kt == 0, kt == KT - 1, ["wq", "hT"], [("ps", 1)])
        C.cp(kTb[:], ps[1][:, 0:NTS], [("ps", 1)], ["kTb"], eng="scalar")
        si = 0
        for (c0, dst) in [(128, kso), (256, vso)]:
            for tt_ in range(2):
                pb = ps[si % 2]
                for kt in range(KT):
                    C.mm(pb[:, 0:128], hT[:, kt, tt_ * 128:(tt_ + 1) * 128], wq[:, kt, c0:c0 + 128], kt == 0, kt == KT - 1,
                         ["wq", "hT"], [("ps", si % 2)])
                C.cp(stg[si % 2][:], pb[:, 0:128], [("ps", si % 2)], [("stg", si % 2)])
                C.st(dst[tt_ * 128:(tt_ + 1) * 128, :], stg[si % 2][:], [("stg", si % 2)], [("o", c0, tt_)], key=("stg", si % 2))
                si += 1

        Kg = C.sb("Kg", [64, 128, 128], BF16)
        Vg = [C.sb(f"Vg{i}", [64, 128, 128], BF16) for i in range(2)]
        KTb = C.sb("KTbig", [128, NPG * 128], BF16)
        kmT = C.sb("kmT", [128, NBLK]); Gs = C.sb("Gs", [8, NBLK]); t8 = C.sb("t8", [8, 8]); bbm = C.sb("bbm", [8, NBLK])
        bbT = C.sb("bbT", [128, 8], BF16)
        Sb = C.sb("Sb", [64, 1024]); PT = C.sb("PTs", [64, 1024], BF16)
        Sn = C.sb("Sn", [8, 8]); Pn = C.sb("Pn", [64, 8], BF16); Vn = C.sb("Vn", [64, 128], BF16)
        rm = C.sb("rm", [128, 4]); negc = C.sb("negc", [128, 1]); rdn = C.sb("rdn", [128, 8])
        attS = C.sb("attS", [128, NTS])
        ptT = C.sb("ptT", [64, 32], I32); idxa = C.sb("idxa", [64, 32, 8], I32)
        pairm = C.sb("pairm", [64, NBLK], BF16); indJ = C.sb("indJ", [128, 64], BF16)
        C.memset(bbT[:], 0.0, ["bbT"]); C.memset(Pn[:], 0.0, ["Pn"]); C.memset(Vn[:], 0.0, ["Vn"])
        C.memset(rm[:], -BIG, ["rm0"])
        C.ld(ptT[:], C.ins["ptabT"], ["ptT"])
        C.ld(pairm[:], C.ins["c_pair"], ["pairm"], eng="gpsimd")
        C.ld(indJ[:], C.ins["c_indj"], ["indJ"], eng="gpsimd")
        for c in range(8):
            C.ts(idxa[:, :, c], ptT[:, :], 8, ALU.mult, ["ptT"], ["idxa"], s2=c, op1=ALU.add)
        ck8 = ck.rearrange("(n c r) d -> (n c) (r d)", c=8, r=16)
        cv8 = cv.rearrange("(n c r) d -> (n c) (r d)", c=8, r=16)

        def gather(dst, src8, b_):
            for c in range(8):
                P.dma("gpsimd",
                      (lambda e, o=dst[:, c * 16:(c + 1) * 16, :].rearrange("p r d -> p (r d)"), ix=idxa[:, b_, c:c + 1]:
                       e.indirect_dma_start(out=o, out_offset=None, in_=src8,
                                            in_offset=bass.IndirectOffsetOnAxis(ap=ix, axis=0))),
                      ["idxa"], [dst_key(dst, c)], key=("gath", id(dst) % 97, c))

        keymap = {}

        def dst_key(dst, c):
            return (keymap[id(dst)], c)

        keymap[id(Kg)] = "Kg"; keymap[id(Vg[0])] = "Vg0"; keymap[id(Vg[1])] = "Vg1"

        for b in range(nb):
            vb = Vg[b % 2]; vkeys = [("Vg%d" % (b % 2), c) for c in range(8)]; kkeys = [("Kg", c) for c in range(8)]
            gather(Kg, ck8, b)
            gather(vb, cv8, b)
            for g16 in range(8):
                for rr in range(16):
                    r_ = g16 * 16 + rr
                    C.tr(psb[:, rr * 64:(rr + 1) * 64], Kg[:, r_, :], identb[0:64, 0:64], [("Kg", r_ // 16), "identb"], [("psb",)])
                C.cp(KTb[:, g16 * 1024:(g16 + 1) * 1024], psb[:, :], [("psb",)], [("KTb", g16)], eng=("vector" if g16 % 2 else "scalar"))
            for r_ in range(128):
                C.mm(ps[3][:, 0:NBLK], Kg[:, r_, :], pairm[:, :], r_ == 0, r_ == 127, [("Kg", r_ // 16), "pairm"], [("ps", 3)])
            C.ts(kmT[:], ps[3][:, 0:NBLK], 1.0 / 256.0, ALU.mult, [("ps", 3)], ["kmT"])
            C.mm(ps[4][0:8, 0:NBLK], qf[:, b * 8:(b + 1) * 8], kmT[:], True, True, ["qf", "kmT"], [("ps", 4)])
            C.cp(Gs[:], ps[4][0:8, 0:NBLK], [("ps", 4)], ["Gs"])
            P.op("vector", lambda e, o=t8[:], i=Gs[:]: e.max(o, i), ["Gs"], ["t8"])
            C.ts(bbm[:], Gs[:], t8[:, 2:3], ALU.is_ge, ["Gs", "t8"], ["bbm"])
            C.ts(bbm[:], bbm[:], BIG, ALU.mult, ["bbm"], ["bbm"], s2=-BIG, op1=ALU.add)
            C.tr(ps[4][0:NBLK, 64:72], bbm[:], ident[0:8, 0:8], ["bbm", "ident"], [("ps", 4)])
            C.cp(bbT[0:NBLK, :], ps[4][0:NBLK, 64:72], [("ps", 4)], ["bbT"], eng="scalar")
            for r_ in range(128):
                pb = ps[5 + r_ // 64]; pk = ("ps", 5 + r_ // 64); cc = (r_ % 64) * 8
                C.mm(pb[0:64, cc:cc + 8], KTb[:, r_ * 64:(r_ + 1) * 64], qb[:, b * 8:(b + 1) * 8], True, False,
                     [("KTb", r_ // 16), "qb"], [pk])
                C.mm(pb[0:64, cc:cc + 8], indJ[:, :], bbT[:], False, True, ["indJ", "bbT"], [pk])
            C.mm(ps[4][0:8, 80:88], kTb[:, b * 8:(b + 1) * 8], qb[:, b * 8:(b + 1) * 8], True, True, ["kTb", "qb"], [("ps", 4)])
            for hh in range(2):
                C.tt(Sb[:, hh * 512:(hh + 1) * 512], ps[5 + hh][0:64, :], alib[0:64, hh * 512:(hh + 1) * 512], ALU.add,
                     [("ps", 5 + hh), "alib"], [("Sb", hh)])
            C.tt(Sn[:], ps[4][0:8, 80:88], nbias[0:8, :], ALU.add, [("ps", 4), "nbias"], ["Sn"])
            C.red(rm[0:64, 0:1], Sb[:], ALU.max, [("Sb", 0), ("Sb", 1)], ["rm0"])
            C.red(rm[0:8, 1:2], Sn[:], ALU.max, ["Sn"], ["rm1"])
            C.tt(rm[0:8, 0:1], rm[0:8, 0:1], rm[0:8, 1:2], ALU.max, ["rm0", "rm1"], ["rm0"])
            C.tr(ps[4][0:1, 128:256], rm[:, 0:1], ident[:], ["rm0", "ident"], [("ps", 4)])
            C.red(rm[0:1, 2:3], ps[4][0:1, 128:256], ALU.max, [("ps", 4)], ["rm2"])
            C.ts(rm[0:1, 2:3], rm[0:1, 2:3], -1.0, ALU.mult, ["rm2"], ["rm2"])
            C.mm(ps[4][:, 256:258], onesf[0:1, :], rm[0:1, 2:4], True, True, ["onesf", "rm2"], [("ps", 4)])
            C.cp(negc[:], ps[4][:, 256:257], [("ps", 4)], ["negc"])
            for hh in range(2):
                C.act(PT[:, hh * 512:(hh + 1) * 512], Sb[:, hh * 512:(hh + 1) * 512], ACT.Exp, [("Sb", hh), "negc"], [("PT", hh)],
                      bias=negc[0:64, 0:1], scale=1.0)
            C.act(Pn[0:8, :], Sn[:], ACT.Exp, ["Sn", "negc"], ["Pn"], bias=negc[0:8, 0:1], scale=1.0)
            for kt in range(KT):
                C.mm(ps[4][0:8, 384:512], hT[:, kt, b * 8:(b + 1) * 8], wq[:, kt, 256:384], kt == 0, kt == KT - 1, ["wq", "hT"],
                     [("ps", 4)])
            C.cp(Vn[0:8, :], ps[4][0:8, 384:512], [("ps", 4)], ["Vn"], eng="scalar")
            po = ps[b % 2]; pok = ("ps", b % 2)
            for r_ in range(128):
                C.mm(po[:, 0:8], vb[:, r_, :], PT[:, r_ * 8:(r_ + 1) * 8], r_ == 0, False, [("Vg%d" % (b % 2), r_ // 16), ("PT", r_ // 64)],
                     [pok])
            C.mm(po[:, 0:8], Vn[:], Pn[:], False, True, ["Vn", "Pn"], [pok])
            for r_ in range(128):
                C.mm(po[:, 8:16], onesb[0:64, :], PT[:, r_ * 8:(r_ + 1) * 8], r_ == 0, False, ["onesb", ("PT", r_ // 64)], [pok])
            C.mm(po[:, 8:16], onesb[0:64, :], Pn[:], False, True, ["onesb", "Pn"], [pok])
            C.recip(rdn[:], po[:, 8:16], [pok], ["rdn"])
            C.tt(attS[:, b * 8:(b + 1) * 8], po[:, 0:8], rdn[:], ALU.mult, [pok, "rdn"], ["attS"])
        if nb < 32:
            C.memset(attS[:, nb * 8:NTS], 0.0, ["attS"])
        C.st(attT, attS[:], ["attS"], ["attT"], key=("attst",))
        P.finish()
    return nc


def _slopes():
    return (2.0 ** (-8.0 * np.arange(1, 9, dtype=np.float32) / 8.0)).astype(np.float32)


def _bf(x):
    return x.astype(ml_dtypes.bfloat16).astype(np.float32)


def _consts_main(half):
    c = {}
    c["c_ident"] = np.eye(128, dtype=np.float32)
    s = np.arange(128)[:, None]; t = np.arange(64)[None, :]
    U = np.zeros((128, 64), np.float32); U[:64] = (s[:64] <= t).astype(np.float32)
    c["c_U"] = U
    neg = np.zeros((128, 64), np.float32); neg[:64] = np.where(t > s[:64], -BIG, 0.0).astype(np.float32)
    c["c_neg"] = neg
    p = np.arange(128)[:, None]; j = np.arange(256)[None, :]
    c["c_cm"] = np.concatenate([np.where(j >= p, 0.0, -BIG), np.where(j >= p + 128, 0.0, -BIG)], axis=1).astype(np.float32)
    sl = _slopes()
    ab = np.zeros((128, 128), np.float32)
    for h in range(8):
        for di in range(16):
            ab[:, h * 16 + di] = sl[h] * (np.arange(128, dtype=np.float32) + 128.0 * (di - 14))
    c["c_abase"] = ab
    gb = np.zeros((128, 32), np.float32)
    for qg in range(4):
        cc = 4 + qg
        for n in range(8):
            if n >= cc or (n < 4 and half == 0):
                gb[:, qg * 8 + n] = -BIG
    c["c_gb"] = gb
    aq = np.zeros((8, 3, 256), np.float32)
    for h in range(8):
        v = (-sl[h] * np.arange(256, dtype=np.float32)).astype(np.float32)
        hi = _bf(v); mid = _bf(v - hi); lo = _bf(v - hi - mid)
        aq[h, 0], aq[h, 1], aq[h, 2] = hi, mid, lo
    c["c_aq"] = aq
    ind = np.zeros((128, 8 * 128), np.float32)
    for n in range(8):
        ind[n, n * 128:(n + 1) * 128] = 1.0
        ind[8:11, n * 128:(n + 1) * 128] = 1.0
    c["c_ind"] = ind
    sel = np.zeros((16, 16 * 128), np.float32)
    for e in range(16):
        sel[e, e * 128:(e + 1) * 128] = 1.0
    c["c_sel"] = sel
    fl = np.zeros((128, 8), np.float32); fl[:, 0] = float(half)
    c["flags"] = fl
    return c


def _fm(v):
    return np.ascontiguousarray(v.reshape(-1, 128).T)


def _prep_main(inp, core, atts_all):
    b, half = core // 2, core % 2
    m = {}
    xpb = inp["x_prompt"][b]
    m["xo"] = np.ascontiguousarray(xpb[half * 1024:(half + 1) * 1024].T)
    m["xp"] = np.ascontiguousarray(xpb[0:1024].T) if half == 1 else np.zeros((D, NPRE), np.float32)
    m["xs"] = np.ascontiguousarray(inp["x_sample"][4 * core:4 * core + 4].reshape(32, D).T)
    m["cT"] = np.ascontiguousarray(np.concatenate([inp["c_prompt"][b:b + 1], inp["c_sample"][4 * core:4 * core + 4]], 0).T)
    m["wmod"] = inp["w_mod"][0]
    m["bmodT"] = _fm(inp["b_mod"][0])
    m["gvec"] = np.concatenate([_fm(inp["g_mix"][0]), _fm(inp["g_ffn"][0]), _fm(inp["g_final"])], axis=1)
    m["win"] = inp["w_in"][0]; m["wout"] = inp["w_out"][0]
    m["wr"] = np.ascontiguousarray(np.concatenate([inp["w_grp"][0], inp["w_exp"][0]], axis=1))
    row = np.concatenate([inp["b_ig"][0], inp["b_fg"][0], inp["b_grp"][0], inp["b_exp"][0], inp["ml_gain"][0]])
    m["rowc"] = np.ascontiguousarray(np.broadcast_to(row[None, :], (128, row.size))).astype(np.float32)
    m["wg"] = inp["w_gate"][0]; m["wu"] = inp["w_up"][0]; m["wd"] = inp["w_down"][0]
    m["atts"] = np.ascontiguousarray(atts_all[32 * core:32 * core + 32].T)
    sc = inp["state_C"][0, 4 * core:4 * core + 4]
    m["sC"] = np.ascontiguousarray(np.swapaxes(sc, -1, -2))
    m["sn"] = np.ascontiguousarray(inp["state_n"][0, 4 * core:4 * core + 4])
    smv = inp["state_m"][0, 4 * core:4 * core + 4].reshape(16)
    m["sm"] = np.ascontiguousarray(np.broadcast_to(smv[None, :], (128, 16))).astype(np.float32)
    m.update(_consts_main(half))
    return {k: np.ascontiguousarray(v, dtype=np.float32) for k, v in m.items()}


def _post_main(res, outs):
    (y_p, y_s, k_p, v_p, C_p, n_p, m_p, C_s, n_s, m_s) = outs
    for core, r in res.items():
        b, half = core // 2, core % 2
        sl = slice(half * 1024, (half + 1) * 1024)
        y_p[b, sl] = r["yo"].T
        y_s[4 * core:4 * core + 4] = r["ys"].T.reshape(4, 8, D)
        k_p[0, b, sl] = r["ko"].T.reshape(1024, 8, 128)
        v_p[0, b, sl] = r["vo"].reshape(1024, 8, 128)
        if half == 1:
            C_p[0, b] = np.swapaxes(r["Cp"], -1, -2)
            n_p[0, b] = r["npo"]
            m_p[0, b] = r["mpo"][0]
        C_s[0, 4 * core:4 * core + 4] = np.swapaxes(r["Cs"], -1, -2)
        n_s[0, 4 * core:4 * core + 4] = r["nso"]
        m_s[0, 4 * core:4 * core + 4] = r["mso"][0].reshape(4, 4)


def _consts_samp(h):
    c = {}
    c["c_ident"] = np.eye(128, dtype=np.float32)
    pair = np.zeros((64, NBLK), np.float32); indj = np.zeros((128, 64), np.float32)
    for j in range(64):
        pair[j, j // 2] = 1.0; indj[j // 2, j] = 1.0
    c["c_pair"] = pair; c["c_indj"] = indj
    sl = float(_slopes()[h])
    j = np.arange(64, dtype=np.float32)[:, None, None]; r = np.arange(128, dtype=np.float32)[None, :, None]
    q = np.arange(8, dtype=np.float32)[None, None, :]
    al = np.zeros((128, 1024), np.float32)
    al[:64] = (sl * (j * 128.0 + r - (8192.0 + q))).reshape(64, 1024)
    c["c_alib"] = al
    nb = np.full((128, 8), -BIG, np.float32)
    for s_ in range(8):
        for q_ in range(8):
            if s_ <= q_:
                nb[s_, q_] = sl * (s_ - q_)
    c["c_nb"] = nb
    return c


def _prep_samp(inp, h):
    m = {}
    m["xs"] = np.ascontiguousarray(inp["x_sample"].reshape(NTS, D).T)
    m["cT"] = np.ascontiguousarray(inp["c_sample"].T)
    m["wmod"] = np.ascontiguousarray(inp["w_mod"][0][:, :4096])
    m["bmodT"] = _fm(inp["b_mod"][0][:4096])
    m["gmix"] = _fm(inp["g_mix"][0])
    w = inp["w_in"][0]
    m["wqkv"] = np.ascontiguousarray(np.concatenate([w[:, h * 128:(h + 1) * 128], w[:, 1024 + h * 128:1024 + (h + 1) * 128],
                                                     w[:, 2048 + h * 128:2048 + (h + 1) * 128]], axis=1))
    m["ck"] = np.ascontiguousarray(inp["cache_k"][0][:, :, h, :]).reshape(2560 * 128, 128)
    m["cv"] = np.ascontiguousarray(inp["cache_v"][0][:, :, h, :]).reshape(2560 * 128, 128)
    m.update(_consts_samp(h))
    m = {k: np.ascontiguousarray(v, dtype=np.float32) for k, v in m.items()}
    m["ptabT"] = np.ascontiguousarray(inp["page_table"].T.astype(np.int32))
    return m


_CACHE = {}


def _get(name, fn):
    if name not in _CACHE:
        _CACHE[name] = fn()
    return _CACHE[name]


def kernel(**inputs):
    inp = {k: np.asarray(v) for k, v in inputs.items()}
    n = 8
    nc1 = _get("samp", lambda: build_samp(nb=32))
    maps1 = [_prep_samp(inp, h) for h in range(n)]
    res1 = run_bass_kernel_spmd(nc1, maps1, core_ids=list(range(n))).results
    del maps1
    atts_all = np.zeros((NTS, 1024), np.float32)
    k_s = np.zeros((1, 32, 8, 8, 128), np.float32); v_s = np.zeros((1, 32, 8, 8, 128), np.float32)
    for h in range(n):
        atts_all[:, h * 128:(h + 1) * 128] = res1[h]["attT"].T
        k_s[0, :, :, h, :] = res1[h]["kso"].reshape(32, 8, 128)
        v_s[0, :, :, h, :] = res1[h]["vso"].reshape(32, 8, 128)
    nc2 = _get("main", lambda: build_main())
    maps2 = [_prep_main(inp, c, atts_all) for c in range(n)]
    res2 = run_bass_kernel_spmd(nc2, maps2, core_ids=list(range(n))).results
    del maps2
    y_p = np.zeros((4, 2048, D), np.float32); y_s = np.zeros((32, 8, D), np.float32)
    k_p = np.zeros((1, 4, 2048, 8, 128), np.float32); v_p = np.zeros((1, 4, 2048, 8, 128), np.float32)
    C_p = np.zeros((1, 4, 4, 256, 256), np.float32); n_p = np.zeros((1, 4, 4, 256), np.float32); m_p = np.zeros((1, 4, 4), np.float32)
    C_s = np.zeros((1, 32, 4, 256, 256), np.float32); n_s = np.zeros((1, 32, 4, 256), np.float32); m_s = np.zeros((1, 32, 4), np.float32)
    _post_main({c: res2[c] for c in range(n)}, (y_p, y_s, k_p, v_p, C_p, n_p, m_p, C_s, n_s, m_s))
    return (y_p, y_s, k_p, v_p, k_s, v_s, C_p, n_p, m_p, C_s, n_s, m_s)
```
